# Optimizing a Trainium2 kernel written in Bass

```python
import jax
import jax.numpy as jnp
from jax import lax
import numpy as np

D_MODEL = 1024
BATCH = 8
SEQ = 2048
DEPTH = 4

HGRN_HEADS = 4
HGRN_DK = 128
HGRN_DV = 128
HGRN_CHUNK = 64
HGRN_KW = HGRN_HEADS * HGRN_DK
HGRN_VW = HGRN_HEADS * HGRN_DV
MLA_HEADS = 8
MLA_Q_RANK = 384
MLA_KV_RANK = 256
MLA_NOPE = 64
MLA_ROPE = 32
MLA_DV = 64
MLA_VW = MLA_HEADS * MLA_DV
ROPE_THETA = 10000.0
Q_BLOCK = 128
N_EXPERTS = 32
TOP_K = 4
D_EXPERT = 1024
SWIGLU_LIMIT = 7.0
SWIGLU_ALPHA = 1.702
MOE_BLOCK = 256
N_BRANCHES = 2
NORM_EPS = 1e-6
IN_SPLITS = (HGRN_KW, HGRN_KW, HGRN_KW, HGRN_VW, HGRN_VW, MLA_Q_RANK, MLA_KV_RANK, MLA_ROPE, N_BRANCHES * D_MODEL)
IN_WIDTH = 3 * HGRN_KW + 2 * HGRN_VW + MLA_Q_RANK + MLA_KV_RANK + MLA_ROPE + N_BRANCHES * D_MODEL

kernel_name = "hybrid_hgrn2_mla_moe_adaln_encoder"


def rms_norm(x, gain):
    xf = x.astype(jnp.float32)
    y = xf * lax.rsqrt(jnp.mean(jnp.square(xf), axis=-1, keepdims=True) + NORM_EPS)
    return (y * gain.astype(jnp.float32)).astype(x.dtype)


def rope_tables(positions):
    inv_freq = ROPE_THETA ** (-jnp.arange(0, MLA_ROPE, 2, dtype=jnp.float32) / MLA_ROPE)
    ang = positions.astype(jnp.float32)[..., None] * inv_freq
    return jnp.cos(ang), jnp.sin(ang)


def apply_rope(x, cos, sin):
    half = x.shape[-1] // 2
    xf = x.astype(jnp.float32)
    x1, x2 = xf[..., :half], xf[..., half:]
    return jnp.concatenate([x1 * cos - x2 * sin, x2 * cos + x1 * sin], axis=-1).astype(x.dtype)


def hgrn2_chunk_scan(q, k, v, log_f):
    B, H, L, DK = q.shape
    DV = v.shape[-1]
    n = L // HGRN_CHUNK

    def to_chunks(a):
        return a.reshape(B, H, n, HGRN_CHUNK, a.shape[-1]).transpose(2, 0, 1, 3, 4)

    mask = jnp.tril(jnp.ones((HGRN_CHUNK, HGRN_CHUNK), dtype=bool))[:, :, None]

    def step(state, inp):
        q_c, k_c, v_c, g_c = inp
        b = jnp.cumsum(g_c, axis=-2)
        diff = b[..., :, None, :] - b[..., None, :, :]
        decay = jnp.exp(jnp.where(mask, diff, -jnp.inf))
        scores = jnp.einsum('bhtd,bhsd,bhtsd->bhts', q_c, k_c, decay)
        o = (jnp.einsum('bhts,bhsv->bhtv', scores, v_c)
             + jnp.einsum('bhtd,bhdv->bhtv', q_c * jnp.exp(b), state))
        b_last = b[..., -1:, :]
        state = (jnp.exp(b_last[..., 0, :])[..., None] * state
                 + jnp.einsum('bhsd,bhsv->bhdv', k_c * jnp.exp(b_last - b), v_c))
        return state, o

    s0 = jnp.zeros((B, H, DK, DV), jnp.float32)
    _, o = lax.scan(step, s0, (to_chunks(q), to_chunks(k), to_chunks(v), to_chunks(log_f)))
    return o.transpose(1, 2, 0, 3, 4).reshape(B, H, L, DV)


def hgrn2_branch(q_in, f_fw_in, f_bw_in, i_in, g_in, lb_fw, lb_bw, out_gain):
    B, S, _ = q_in.shape

    def heads(a, d):
        return a.reshape(B, S, HGRN_HEADS, d).transpose(0, 2, 1, 3).astype(jnp.float32)

    q = heads(jax.nn.silu(q_in.astype(jnp.float32)), HGRN_DK)
    v = heads(i_in, HGRN_DV)

    def forget(f_in, lb):
        f = lb + (1.0 - lb) * jax.nn.sigmoid(f_in.astype(jnp.float32))
        return heads(1.0 - f, HGRN_DK), heads(jnp.log(f), HGRN_DK)

    k_fw, lf_fw = forget(f_fw_in, lb_fw)
    k_bw, lf_bw = forget(f_bw_in, lb_bw)
    flip = lambda a: jnp.flip(a, axis=2)
    o = (hgrn2_chunk_scan(q, k_fw, v, lf_fw)
         + flip(hgrn2_chunk_scan(flip(q), flip(k_bw), flip(v), flip(lf_bw))))
    o = rms_norm(o, out_gain)
    o = o.transpose(0, 2, 1, 3).reshape(B, S, HGRN_VW)
    return (o * jax.nn.sigmoid(g_in.astype(jnp.float32))).astype(q_in.dtype)


def mla_branch(c_q, c_kv, k_pe, cos, sin, q_norm_gain, w_q_up, kv_norm_gain, w_kv_up):
    B, S, _ = c_q.shape
    q = (rms_norm(c_q, q_norm_gain) @ w_q_up).reshape(B, S, MLA_HEADS, MLA_NOPE + MLA_ROPE)
    q_nope, q_pe = q[..., :MLA_NOPE], q[..., MLA_NOPE:]
    q_pe = apply_rope(q_pe, cos[:, :, None, :], sin[:, :, None, :])
    kv = (rms_norm(c_kv, kv_norm_gain) @ w_kv_up).reshape(B, S, MLA_HEADS, MLA_NOPE + MLA_DV)
    k_nope, v = kv[..., :MLA_NOPE], kv[..., MLA_NOPE:]
    k_pe = apply_rope(k_pe, cos, sin)
    n_blocks = S // Q_BLOCK
    scale = (MLA_NOPE + MLA_ROPE) ** -0.5

    def to_blocks(a):
        return a.reshape(B, n_blocks, Q_BLOCK, *a.shape[2:]).swapaxes(0, 1)

    def attend(blk):
        qn, qp = blk
        s = (jnp.einsum('bqhd,bkhd->bhqk', qn, k_nope)
             + jnp.einsum('bqhr,bkr->bhqk', qp, k_pe))
        p = jax.nn.softmax(s.astype(jnp.float32) * scale, axis=-1)
        return jnp.einsum('bhqk,bkhv->bqhv', p.astype(v.dtype), v)

    o = lax.map(attend, (to_blocks(q_nope), to_blocks(q_pe)))
    return o.swapaxes(0, 1).reshape(B, S, MLA_VW)


def hybrid_mixer(h, cos, sin, w_in, lb_fw, lb_bw, hgrn_gain, w_out_hgrn,
                 q_norm_gain, w_q_up, kv_norm_gain, w_kv_up, w_out_mla, w_o):
    proj = h @ w_in
    split_at = np.cumsum(IN_SPLITS)[:-1].tolist()
    q_a, f_fw, f_bw, i_a, g_a, c_q, c_kv, k_pe, gates = jnp.split(proj, split_at, axis=-1)
    y_a = hgrn2_branch(q_a, f_fw, f_bw, i_a, g_a, lb_fw, lb_bw, hgrn_gain) @ w_out_hgrn
    y_b = mla_branch(c_q, c_kv, k_pe, cos, sin, q_norm_gain, w_q_up, kv_norm_gain, w_kv_up) @ w_out_mla
    gate_a, gate_b = jnp.split(gates, N_BRANCHES, axis=-1)
    y = jax.nn.sigmoid(gate_a) * y_a + jax.nn.sigmoid(gate_b) * y_b
    return y @ w_o


def moe_ffn(h, w_router, b_router, w_gate_up, b_gate_up, w_down, b_down):
    T, D = h.shape
    logits = (h @ w_router + b_router).astype(jnp.float32)
    top_val, top_idx = lax.top_k(logits, TOP_K)
    top_w = jax.nn.softmax(top_val, axis=-1)
    A = T * TOP_K
    flat_e = top_idx.reshape(A)
    order = jnp.argsort(flat_e)
    e_sorted = flat_e[order]
    tok_sorted = order // TOP_K
    w_sorted = top_w.reshape(A)[order]
    counts = jnp.bincount(flat_e, length=N_EXPERTS)
    padded = (counts + MOE_BLOCK - 1) // MOE_BLOCK * MOE_BLOCK
    pad_end = jnp.cumsum(padded)
    pad_start = pad_end - padded
    start = jnp.cumsum(counts) - counts
    dest = pad_start[e_sorted] + jnp.arange(A, dtype=jnp.int32) - start[e_sorted]
    n_blocks = -(-(A + N_EXPERTS * (MOE_BLOCK - 1)) // MOE_BLOCK)
    rows = n_blocks * MOE_BLOCK
    x_rows = jnp.zeros((rows, D), h.dtype).at[dest].set(h[tok_sorted])
    block_start = jnp.arange(n_blocks, dtype=jnp.int32) * MOE_BLOCK
    block_e = jnp.minimum(jnp.searchsorted(pad_end, block_start, side='right'), N_EXPERTS - 1)

    def expert_block(args):
        xb, e = args
        gu = xb @ w_gate_up[e] + b_gate_up[e]
        gate = jnp.minimum(gu[:, 0::2], SWIGLU_LIMIT)
        up = jnp.clip(gu[:, 1::2], -SWIGLU_LIMIT, SWIGLU_LIMIT)
        act = (up + 1.0) * gate * jax.nn.sigmoid(SWIGLU_ALPHA * gate)
        return act @ w_down[e] + b_down[e]

    y_rows = lax.map(expert_block, (x_rows.reshape(n_blocks, MOE_BLOCK, D), block_e)).reshape(rows, D)
    contrib = y_rows[dest].astype(jnp.float32) * w_sorted[:, None]
    out = jnp.zeros((T, D), jnp.float32).at[tok_sorted].add(contrib)
    return out.astype(h.dtype)


def setup_inputs(seed: int = 0) -> dict:
    key = jax.random.key(seed)
    ks = jax.random.split(key, 24)

    def nrm(k, shape, scale):
        return jax.random.normal(k, shape, jnp.float32) * scale

    D = D_MODEL
    return {
        "x": nrm(ks[0], (BATCH, SEQ, D), 1.0),
        "c": nrm(ks[1], (BATCH, D), 1.0),
        "positions": (jnp.arange(SEQ, dtype=jnp.int32)[None, :]
                      + jax.random.randint(ks[2], (BATCH, 1), 0, SEQ, dtype=jnp.int32)),
        "w_ada": nrm(ks[3], (DEPTH, D, 6 * D), 0.5 * D ** -0.5),
        "b_ada": nrm(ks[4], (DEPTH, 6 * D), 0.02),
        "norm_mix_gain": 1.0 + nrm(ks[5], (DEPTH, D), 0.02),
        "norm_ffn_gain": 1.0 + nrm(ks[6], (DEPTH, D), 0.02),
        "w_in": nrm(ks[7], (DEPTH, D, IN_WIDTH), D ** -0.5),
        "hgrn_lb_logits": nrm(ks[8], (DEPTH, 2, HGRN_KW), 0.5),
        "hgrn_out_norm_gain": 1.0 + nrm(ks[9], (DEPTH, HGRN_DV), 0.02),
        "w_out_hgrn": nrm(ks[10], (DEPTH, HGRN_VW, D), HGRN_VW ** -0.5),
        "mla_q_norm_gain": 1.0 + nrm(ks[11], (DEPTH, MLA_Q_RANK), 0.02),
        "w_q_up": nrm(ks[12], (DEPTH, MLA_Q_RANK, MLA_HEADS * (MLA_NOPE + MLA_ROPE)), MLA_Q_RANK ** -0.5),
        "mla_kv_norm_gain": 1.0 + nrm(ks[13], (DEPTH, MLA_KV_RANK), 0.02),
        "w_kv_up": nrm(ks[14], (DEPTH, MLA_KV_RANK, MLA_HEADS * (MLA_NOPE + MLA_DV)), MLA_KV_RANK ** -0.5),
        "w_out_mla": nrm(ks[15], (DEPTH, MLA_VW, D), MLA_VW ** -0.5),
        "w_o": nrm(ks[16], (DEPTH, D, D), D ** -0.5),
        "w_router": nrm(ks[17], (DEPTH, D, N_EXPERTS), D ** -0.5),
        "b_router": nrm(ks[18], (DEPTH, N_EXPERTS), 0.01),
        "w_gate_up": nrm(ks[19], (DEPTH, N_EXPERTS, D, 2 * D_EXPERT), D ** -0.5),
        "b_gate_up": nrm(ks[20], (DEPTH, N_EXPERTS, 2 * D_EXPERT), 0.02),
        "w_down": nrm(ks[21], (DEPTH, N_EXPERTS, D_EXPERT, D), D_EXPERT ** -0.5),
        "b_down": nrm(ks[22], (DEPTH, N_EXPERTS, D), 0.02),
        "final_norm_gain": 1.0 + nrm(ks[23], (D,), 0.02),
    }


def reference(x, c, positions, w_ada, b_ada, norm_mix_gain, norm_ffn_gain, w_in, hgrn_lb_logits,
              hgrn_out_norm_gain, w_out_hgrn, mla_q_norm_gain, w_q_up, mla_kv_norm_gain, w_kv_up,
              w_out_mla, w_o, w_router, b_router, w_gate_up, b_gate_up, w_down, b_down,
              final_norm_gain):
    B, S, D = x.shape
    cos, sin = rope_tables(positions)
    lb = jnp.cumsum(jax.nn.softmax(hgrn_lb_logits.astype(jnp.float32), axis=0), axis=0)
    lb = lb - lb[0]
    cond = jax.nn.silu(c)
    for l in range(DEPTH):
        mod = cond @ w_ada[l] + b_ada[l]
        sh1, sc1, g1, sh2, sc2, g2 = jnp.split(mod[:, None, :], 6, axis=-1)
        h = rms_norm(x, norm_mix_gain[l]) * (1.0 + sc1) + sh1
        x = x + g1 * hybrid_mixer(h, cos, sin, w_in[l], lb[l, 0], lb[l, 1], hgrn_out_norm_gain[l],
                                  w_out_hgrn[l], mla_q_norm_gain[l], w_q_up[l], mla_kv_norm_gain[l],
                                  w_kv_up[l], w_out_mla[l], w_o[l])
        h = rms_norm(x, norm_ffn_gain[l]) * (1.0 + sc2) + sh2
        x = x + g2 * moe_ffn(h.reshape(B * S, D), w_router[l], b_router[l], w_gate_up[l],
                             b_gate_up[l], w_down[l], b_down[l]).reshape(B, S, D)
    return rms_norm(x, final_norm_gain)
```

```python
import numpy as np
import os
from contextlib import ExitStack
import concourse.bass as bass
import concourse.mybir as mybir
from concourse.bass_utils import run_bass_kernel_spmd

F32 = mybir.dt.float32
BF16 = mybir.dt.bfloat16
I32 = mybir.dt.int32
AF = mybir.ActivationFunctionType
ALU = mybir.AluOpType
AX = mybir.AxisListType

S = 2048
D = 1024
NT = 16
NB = 4
KD = 8
E = 32
INW = 5280
EPS = 1e-6


class Buf:
    __slots__ = ("name", "writer", "readers", "dsem", "dcnt")

    def __init__(self, name):
        self.name = name
        self.writer = None
        self.readers = {}
        self.dsem = None
        self.dcnt = 0


class Sched:
    def __init__(self, nc, es):
        self.nc = nc
        self.es = es
        self.E = {"pe": nc.tensor, "act": nc.scalar, "dve": nc.vector, "pool": nc.gpsimd, "sp": nc.sync}
        self.sem = {k: es.enter_context(nc.semaphore("s_" + k)) for k in self.E}
        self.cnt = {k: 0 for k in self.E}
        self.seen = {k: {} for k in self.E}
        self.nbuf = 0
        self.dtoks = {}
        self.dsems = {}

    def buf(self, name=None):
        self.nbuf += 1
        return Buf(name or f"b{self.nbuf}")

    def _wait(self, eng, tok):
        sem, val = tok
        if eng == "pe" and sem is self.sem["pe"]:
            return
        key = id(sem)
        if self.seen[eng].get(key, 0) < val:
            self.E[eng].wait_ge(sem, val)
            self.seen[eng][key] = val

    def _deps(self, eng, reads, writes):
        for b in reads:
            if b.writer is not None:
                self._wait(eng, b.writer)
        for b in writes:
            if b.writer is not None:
                self._wait(eng, b.writer)
            for tok in list(b.readers.values()):
                self._wait(eng, tok)

    def _commit(self, tok, reads, writes):
        k = id(tok[0])
        for b in reads:
            if k not in b.readers or b.readers[k][1] < tok[1]:
                b.readers[k] = tok
        for b in writes:
            b.writer = tok
            b.readers = {}

    def op(self, eng, fn, reads=(), writes=()):
        self._deps(eng, reads, writes)
        ins = fn(self.E[eng])
        self.cnt[eng] += 1
        ins.then_inc(self.sem[eng], 1)
        tok = (self.sem[eng], self.cnt[eng])
        self._commit(tok, reads, writes)
        return tok

    def dma(self, eng, out, in_, reads=(), writes=()):
        self._deps(eng, reads, writes)
        b = writes[0]
        if b.name not in self.dsems:
            self.dsems[b.name] = [self.es.enter_context(self.nc.semaphore("d_" + b.name)), 0]
        ent = self.dsems[b.name]
        ent[1] += 16
        b.dsem = ent[0]
        b.dcnt = ent[1]
        self.E[eng].dma_start(out=out, in_=in_).then_inc(b.dsem, 16)
        tok = (b.dsem, b.dcnt)
        self.dtoks[id(b.dsem)] = tok
        self._commit(tok, reads, writes)
        return tok

    def barrier(self):
        toks = [(self.sem[k], self.cnt[k]) for k in self.E if self.cnt[k] > 0]
        toks += list(self.dtoks.values())
        for eng in self.E:
            for tok in toks:
                if tok[0] is self.sem[eng]:
                    continue
                self._wait(eng, tok)

    def finish(self, eng, bufs):
        for b in bufs:
            if b.writer is not None:
                self._wait(eng, b.writer)


class T:
    def __init__(self, sc, es, name, shape, dtype, psum=False):
        nc = sc.nc
        base = name
        sc.nbuf += 1
        name = f"{name}_{sc.nbuf}"
        if psum:
            self.t = es.enter_context(nc.psum_tensor(name, shape, dtype))
        else:
            self.t = es.enter_context(nc.sbuf_tensor(name, shape, dtype))
        self.b = sc.buf(base)

    def __getitem__(self, k):
        return self.t[k]


def mixer_layer(nc, sc, TT, nextps, PS, l, xT, xb, hT, ident, modT, gsc1, norm_modulate, dd, dbg):
    norm_modulate(l, gsc1, 0)
    w_in_d = dd["w_in"]

    def wload(dst, c0, n):
        sc.dma("pool", dst[:], w_in_d[l, :, c0:c0 + n].rearrange("(k p) c -> p k c", p=128), writes=[dst.b])

    with ExitStack() as esm:
        ogT = TT("ogT", [128, 4, S], BF16, scope=esm)

        with ExitStack() as eh:
          if dbg in (None, "hgrn"):
                lbc = TT("lbc", [128, 8], F32, scope=eh)
                omlc = TT("omlc", [128, 8], F32, scope=eh)
                omlr = TT("omlr", [128, 1024], F32, scope=eh)
                with ExitStack() as e2:
                    lc = TT("lc", [128, 4, 8], F32, scope=e2)
                    lr = TT("lr", [128, 4, 1024], F32, scope=e2)
                    dc = TT("dc", [128, 8], F32, scope=e2)
                    dr = TT("dr", [128, 1024], F32, scope=e2)
                    sc.dma("sp", lc[:], dd["lbl_col"], writes=[lc.b])
                    sc.dma("sp", lr[:], dd["lbl_row"], writes=[lr.b])
                    for (src, den, num) in ((lc, dc, lbc), (lr, dr, omlr)):
                        sc.op("act", lambda e, src=src: e.activation(out=src[:], in_=src[:], func=AF.Exp), reads=[src.b], writes=[src.b])
                        sc.op("dve", lambda e, src=src, den=den: e.tensor_tensor(out=den[:], in0=src[:, 0, :], in1=src[:, 1, :], op=ALU.add),
                              reads=[src.b], writes=[den.b])
                        for j in (2, 3):
                            sc.op("dve", lambda e, src=src, den=den, j=j: e.tensor_tensor(out=den[:], in0=den[:], in1=src[:, j, :], op=ALU.add),
                                  reads=[src.b, den.b], writes=[den.b])
                        sc.op("dve", lambda e, den=den: e.reciprocal(out=den[:], in_=den[:]), reads=[den.b], writes=[den.b])
                        if l == 0:
                            sc.op("dve", lambda e, num=num: e.memset(num[:], 0.0), writes=[num.b])
                        else:
                            sc.op("dve", lambda e, src=src, num=num: e.tensor_copy(out=num[:], in_=src[:, 1, :]), reads=[src.b], writes=[num.b])
                            for j in range(2, l + 1):
                                sc.op("dve", lambda e, src=src, num=num, j=j: e.tensor_tensor(out=num[:], in0=num[:], in1=src[:, j, :], op=ALU.add),
                                      reads=[src.b, num.b], writes=[num.b])
                            sc.op("dve", lambda e, num=num, den=den: e.tensor_tensor(out=num[:], in0=num[:], in1=den[:], op=ALU.mult),
                                  reads=[num.b, den.b], writes=[num.b])
                    sc.op("dve", lambda e: e.tensor_scalar(out=omlc[:], in0=lbc[:], scalar1=-1.0, scalar2=1.0, op0=ALU.mult, op1=ALU.add),
                          reads=[lbc.b], writes=[omlc.b])
                    sc.op("dve", lambda e: e.tensor_scalar(out=omlr[:], in0=omlr[:], scalar1=-1.0, scalar2=1.0, op0=ALU.mult, op1=ALU.add),
                          reads=[omlr.b], writes=[omlr.b])
                    sc.barrier()

                hg = TT("hg", [128, 128], F32, scope=eh)
                uext = TT("uext_sb", [128, 2, 67], F32, scope=eh)
                msk = TT("mask_sb", [128, 2, 64], F32, scope=eh)
                sc.dma("sp", hg[:], dd["hgain"][:, l, :], writes=[hg.b])
                sc.dma("sp", uext[:], dd["uext"], writes=[uext.b])
                sc.dma("sp", msk[:], dd["mask"], writes=[msk.b])
                wq = TT("wq", [128, KD, 128], BF16, scope=eh)
                wf = [TT(f"wf{i}", [128, KD, 128], BF16, scope=eh) for i in range(2)]
                wi = TT("wi", [128, KD, 128], BF16, scope=eh)
                wgt = TT("wgt", [128, KD, 128], BF16, scope=eh)
                qT = TT("qT", [128, S], F32, scope=eh)
                kT = TT("kT", [128, S], F32, scope=eh)
                gTM = TT("gTM", [128, NT, 128], F32, scope=eh)
                kTM = TT("kTM", [128, NT, 128], F32, scope=eh)
                vTM = TT("vTM", [128, NT, 128], BF16, scope=eh)
                sgTM = TT("sgTM", [128, NT, 128], F32, scope=eh)
                oacc = TT("oacc", [128, NT, 128], F32, scope=eh)
                e1 = [TT(f"e1_{i}", [128, 64], F32, scope=eh) for i in range(2)]
                e2t = [TT(f"e2t_{i}", [128, 64], F32, scope=eh) for i in range(2)]
                e2 = [TT(f"e2_{i}", [128, 128], F32, scope=eh) for i in range(2)]
                em3 = [TT(f"em3_{i}", [128, 3], F32, scope=eh) for i in range(2)]
                QbT = [TT(f"QbT{i}", [128, 64], BF16, scope=eh) for i in range(2)]
                KbT = [TT(f"KbT{i}", [128, 64], BF16, scope=eh) for i in range(2)]
                Kb = [TT(f"Kb{i}", [128, 128], BF16, scope=eh) for i in range(2)]
                ATm = [TT(f"ATm{i}", [128, 64], BF16, scope=eh) for i in range(2)]
                Sst = TT("Sst", [128, 128], F32, scope=eh)
                Sp = [TT(f"Sp{i}", [128, 128], BF16, scope=eh) for i in range(2)]
                ssq = TT("hssq", [128, NT], F32, scope=eh)

                for h in range(4):
                    wload(wq, h * 128, 128)
                    wload(wf[0], 512 + h * 128, 128)
                    wload(wf[1], 1024 + h * 128, 128)
                    wload(wi, 1536 + h * 128, 128)
                    wload(wgt, 2048 + h * 128, 128)
                    for blk in range(NB):
                        ts = slice(blk * 512, (blk + 1) * 512)
                        ps = nextps()

                        def mmq(e, ps=ps, ts=ts):
                            ins = None
                            for k in range(KD):
                                ins = e.matmul(ps[:, :], lhsT=wq[:, k, :], rhs=hT[:, k, ts], start=(k == 0), stop=(k == KD - 1))
                            return ins
                        sc.op("pe", mmq, reads=[wq.b, hT.b], writes=[ps.b])
                        sc.op("act", lambda e, ps=ps, ts=ts: e.activation(out=qT[:, ts], in_=ps[:, :], func=AF.Silu), reads=[ps.b], writes=[qT.b])
                    for t in range(NT):
                        tsl = slice(t * 128, (t + 1) * 128)
                        ps = nextps()

                        def mmv(e, ps=ps, tsl=tsl):
                            ins = None
                            for gi_, wt in enumerate((wi, wgt)):
                                for k in range(KD):
                                    ins = e.matmul(ps[:, gi_ * 128:(gi_ + 1) * 128], lhsT=hT[:, k, tsl], rhs=wt[:, k, :],
                                                   start=(k == 0), stop=(k == KD - 1))
                            return ins
                        sc.op("pe", mmv, reads=[wi.b, wgt.b, hT.b], writes=[ps.b])
                        sc.op("act", lambda e, ps=ps, t=t: e.copy(out=vTM[:, t, :], in_=ps[:, 0:128]), reads=[ps.b], writes=[vTM.b])
                        sc.op("act", lambda e, ps=ps, t=t: e.activation(out=sgTM[:, t, :], in_=ps[:, 128:256], func=AF.Sigmoid),
                              reads=[ps.b], writes=[sgTM.b])
                    for dr_ in range(2):
                        w_f = wf[dr_]
                        col = dr_ * 4 + h
                        rsl = slice(dr_ * 512 + h * 128, dr_ * 512 + (h + 1) * 128)
                        for blk in range(NB):
                            ts = slice(blk * 512, (blk + 1) * 512)
                            ps = nextps()

                            def mmk(e, ps=ps, ts=ts, w_f=w_f):
                                ins = None
                                for k in range(KD):
                                    ins = e.matmul(ps[:, :], lhsT=w_f[:, k, :], rhs=hT[:, k, ts], start=(k == 0), stop=(k == KD - 1))
                                return ins
                            sc.op("pe", mmk, reads=[w_f.b, hT.b], writes=[ps.b])
                            sc.op("act", lambda e, ps=ps, ts=ts: e.activation(out=kT[:, ts], in_=ps[:, :], func=AF.Sigmoid, scale=-1.0),
                                  reads=[ps.b], writes=[kT.b])
                            sc.op("dve", lambda e, ts=ts, col=col: e.tensor_scalar(out=kT[:, ts], in0=kT[:, ts], scalar1=omlc[:, col:col + 1],
                                                                                  scalar2=None, op0=ALU.mult), reads=[kT.b, omlc.b], writes=[kT.b])
                        for t in range(NT):
                            tsl = slice(t * 128, (t + 1) * 128)
                            ps = nextps()

                            def mmf(e, ps=ps, tsl=tsl, w_f=w_f):
                                ins = None
                                for k in range(KD):
                                    ins = e.matmul(ps[:, 0:128], lhsT=hT[:, k, tsl], rhs=w_f[:, k, :], start=(k == 0), stop=(k == KD - 1))
                                return ins
                            sc.op("pe", mmf, reads=[w_f.b, hT.b], writes=[ps.b])
                            sc.op("act", lambda e, ps=ps, t=t: e.activation(out=kTM[:, t, :], in_=ps[:, 0:128], func=AF.Sigmoid, scale=-1.0),
                                  reads=[ps.b], writes=[kTM.b])
                            sc.op("dve", lambda e, t=t, rsl=rsl: e.tensor_tensor(out=kTM[:, t, :], in0=kTM[:, t, :], in1=omlr[:, rsl], op=ALU.mult),
                                  reads=[kTM.b, omlr.b], writes=[kTM.b])
                            sc.op("act", lambda e, t=t: e.activation(out=gTM[:, t, :], in_=kTM[:, t, :], func=AF.Ln, scale=-1.0, bias=1.0),
                                  reads=[kTM.b], writes=[gTM.b])
                        order = list(range(32)) if dr_ == 0 else list(range(31, -1, -1))
                        for a_ in ATm:
                            sc.op("dve", lambda e, a_=a_: e.memset(a_[:], 0.0), reads=[a_.b], writes=[a_.b])
                        for ci, c in enumerate(order):
                            t, half = divmod(c, 2)
                            hr = slice(half * 64, half * 64 + 64)
                            cc = slice(c * 64, (c + 1) * 64)
                            bA = PS[2 * (ci % 4)]
                            bB = PS[2 * (ci % 4) + 1]
                            i2 = ci % 2
                            first = (ci == 0)

                            def mmcs(e, bA=bA, hr=hr, t=t):
                                e.matmul(bA[:, 0:67], lhsT=gTM[hr, t, :], rhs=uext[hr, dr_, :], start=True, stop=True)
                                return e.matmul(bA[hr, 128:256], lhsT=uext[hr, dr_, 0:64], rhs=gTM[hr, t, :], start=True, stop=True)
                            sc.op("pe", mmcs, reads=[gTM.b, uext.b], writes=[bA.b])
                            sc.op("act", lambda e, bA=bA, i2=i2: e.activation(out=e1[i2][:], in_=bA[:, 0:64], func=AF.Exp), reads=[bA.b], writes=[e1[i2].b])
                            sc.op("act", lambda e, bA=bA, i2=i2: e.activation(out=e2t[i2][:], in_=bA[:, 0:64], func=AF.Exp, scale=-1.0),
                                  reads=[bA.b], writes=[e2t[i2].b])
                            sc.op("act", lambda e, bA=bA, i2=i2, hr=hr: e.activation(out=e2[i2][hr, :], in_=bA[hr, 128:256], func=AF.Exp, scale=-1.0),
                                  reads=[bA.b], writes=[e2[i2].b])
                            sc.op("act", lambda e, bA=bA, i2=i2: e.activation(out=em3[i2][:], in_=bA[:, 64:67], func=AF.Exp), reads=[bA.b], writes=[em3[i2].b])
                            sc.op("dve", lambda e, i2=i2, cc=cc: e.tensor_tensor(out=QbT[i2][:], in0=qT[:, cc], in1=e1[i2][:], op=ALU.mult),
                                  reads=[qT.b, e1[i2].b], writes=[QbT[i2].b])
                            sc.op("dve", lambda e, i2=i2, cc=cc: e.tensor_tensor(out=KbT[i2][:], in0=kT[:, cc], in1=e2t[i2][:], op=ALU.mult),
                                  reads=[kT.b, e2t[i2].b], writes=[KbT[i2].b])
                            sc.op("dve", lambda e, i2=i2, hr=hr, t=t: e.tensor_tensor(out=Kb[i2][hr, :], in0=kTM[hr, t, :], in1=e2[i2][hr, :], op=ALU.mult),
                                  reads=[kTM.b, e2[i2].b], writes=[Kb[i2].b])
                            sc.op("pe", lambda e, bA=bA, hr=hr, i2=i2: e.matmul(bA[hr, 256:320], lhsT=KbT[i2][:], rhs=QbT[i2][:], start=True, stop=True),
                                  reads=[KbT[i2].b, QbT[i2].b], writes=[bA.b])
                            sc.op("dve", lambda e, bA=bA, hr=hr, i2=i2: e.copy_predicated(out=ATm[i2][hr, :], mask=msk[hr, dr_, :].bitcast(mybir.dt.uint32), data=bA[hr, 256:320]),
                                  reads=[bA.b, msk.b, ATm[i2].b], writes=[ATm[i2].b])
                            if not first:
                                sc.op("dve", lambda e, i2=i2: e.tensor_scalar(out=Sp[i2][:], in0=Sst[:], scalar1=em3[i2][:, 0:1], scalar2=None, op0=ALU.mult),
                                      reads=[Sst.b, em3[i2].b], writes=[Sp[i2].b])

                            def mmo(e, bB=bB, bA=bA, hr=hr, t=t, i2=i2, first=first):
                                ins = e.matmul(bB[hr, 0:128], lhsT=ATm[i2][hr, :], rhs=vTM[hr, t, :], start=True, stop=first)
                                if not first:
                                    ins = e.matmul(bB[hr, 0:128], lhsT=QbT[i2][:], rhs=Sp[i2][:], start=False, stop=True)
                                ins2 = e.matmul(bA[:, 384:512], lhsT=Kb[i2][hr, :], rhs=vTM[hr, t, :], start=True, stop=True)
                                return ins2
                            sc.op("pe", mmo, reads=[ATm[i2].b, vTM.b, QbT[i2].b, Sp[i2].b, Kb[i2].b], writes=[bB.b, bA.b])
                            if dr_ == 0:
                                sc.op("act", lambda e, bB=bB, hr=hr, t=t: e.copy(out=oacc[hr, t, :], in_=bB[hr, 0:128]), reads=[bB.b], writes=[oacc.b])
                            else:
                                sc.op("dve", lambda e, bB=bB, hr=hr, t=t: e.tensor_tensor(out=oacc[hr, t, :], in0=oacc[hr, t, :], in1=bB[hr, 0:128], op=ALU.add),
                                      reads=[bB.b, oacc.b], writes=[oacc.b])
                            if first:
                                sc.op("dve", lambda e, bA=bA, i2=i2: e.tensor_scalar(out=Sst[:], in0=bA[:, 384:512], scalar1=em3[i2][:, 2:3], scalar2=None, op0=ALU.mult),
                                      reads=[bA.b, em3[i2].b], writes=[Sst.b])
                            else:
                                sc.op("dve", lambda e, i2=i2: e.tensor_scalar(out=Sst[:], in0=Sst[:], scalar1=em3[i2][:, 1:2], scalar2=None, op0=ALU.mult),
                                      reads=[Sst.b, em3[i2].b], writes=[Sst.b])
                                sc.op("dve", lambda e, bA=bA, i2=i2: e.scalar_tensor_tensor(out=Sst[:], in0=bA[:, 384:512], scalar=em3[i2][:, 2:3], in1=Sst[:],
                                                                                            op0=ALU.mult, op1=ALU.add),
                                      reads=[bA.b, em3[i2].b, Sst.b], writes=[Sst.b])
                    sq3 = kTM
                    sc.op("dve", lambda e: e.tensor_tensor(out=sq3[:], in0=oacc[:], in1=oacc[:], op=ALU.mult), reads=[oacc.b], writes=[sq3.b])
                    sc.op("dve", lambda e: e.reduce_sum(out=ssq[:], in_=sq3[:], axis=AX.X), reads=[sq3.b], writes=[ssq.b])
                    sc.op("dve", lambda e: e.tensor_scalar(out=ssq[:], in0=ssq[:], scalar1=1.0 / 128, scalar2=EPS, op0=ALU.mult, op1=ALU.add),
                          reads=[ssq.b], writes=[ssq.b])
                    sc.op("act", lambda e: e.activation(out=ssq[:], in_=ssq[:], func=AF.Sqrt), reads=[ssq.b], writes=[ssq.b])
                    sc.op("dve", lambda e: e.reciprocal(out=ssq[:], in_=ssq[:]), reads=[ssq.b], writes=[ssq.b])
                    sc.op("dve", lambda e: e.tensor_tensor(out=oacc[:], in0=oacc[:], in1=ssq[:].unsqueeze(2).to_broadcast([128, NT, 128]), op=ALU.mult),
                          reads=[oacc.b, ssq.b], writes=[oacc.b])
                    sc.op("dve", lambda e: e.tensor_tensor(out=oacc[:], in0=oacc[:], in1=hg[:].unsqueeze(1).to_broadcast([128, NT, 128]), op=ALU.mult),
                          reads=[oacc.b, hg.b], writes=[oacc.b])
                    sc.op("dve", lambda e: e.tensor_tensor(out=oacc[:], in0=oacc[:], in1=sgTM[:], op=ALU.mult),
                          reads=[oacc.b, sgTM.b], writes=[oacc.b])
                    for tg in range(4):
                        ps = nextps()

                        def trh(e, ps=ps, tg=tg):
                            ins = None
                            for j in range(4):
                                ins = e.transpose(ps[:, j * 128:(j + 1) * 128], oacc[:, tg * 4 + j, :], ident[:])
                            return ins
                        sc.op("pe", trh, reads=[oacc.b, ident.b], writes=[ps.b])
                        sc.op("act", lambda e, ps=ps, tg=tg, h=h: e.copy(out=ogT[:, h, tg * 512:(tg + 1) * 512], in_=ps[:, :]), reads=[ps.b], writes=[ogT.b])
                sc.barrier()

        if dbg == "hgrn":
            sc.op("dve", lambda e: e.tensor_copy(out=xT[:, 0:4, :], in_=ogT[:]), reads=[ogT.b] + xb[0:4], writes=xb[0:4])
            sc.barrier()
            return
        mlaT = TT("mlaT", [128, 4, S], BF16, scope=esm)
        with ExitStack() as em:
            cosT = TT("cosT", [128, S], BF16, scope=em)
            sinT = TT("sinT", [128, S], BF16, scope=em)
            cqT = TT("cqT", [128, 3, S], BF16, scope=em)
            ckvT = TT("ckvT", [128, 2, S], BF16, scope=em)
            kpeT = TT("kpeT", [128, S], BF16, scope=em)
            with ExitStack() as e2:
                posi = TT("posi", [128, S], I32, scope=e2)
                u = TT("ropeu", [128, S], F32, scope=e2)
                nf = TT("ropen", [128, S], F32, scope=e2)
                ni = TT("ropeni", [128, S], I32, scope=e2)
                fq = TT("fq", [128, 1], F32, scope=e2)
                sc.dma("sp", posi[:], dd["pos"], writes=[posi.b])
                sc.dma("sp", fq[:], dd["freq"], writes=[fq.b])
                sc.op("dve", lambda e: e.tensor_copy(out=u[:], in_=posi[:]), reads=[posi.b], writes=[u.b])
                sc.op("dve", lambda e: e.tensor_scalar(out=u[:], in0=u[:], scalar1=fq[:, 0:1], scalar2=None, op0=ALU.mult), reads=[u.b, fq.b], writes=[u.b])
                sc.op("dve", lambda e: e.tensor_copy(out=ni[:], in_=u[:]), reads=[u.b], writes=[ni.b])
                sc.op("dve", lambda e: e.tensor_copy(out=nf[:], in_=ni[:]), reads=[ni.b], writes=[nf.b])
                sc.op("dve", lambda e: e.tensor_tensor(out=u[:], in0=u[:], in1=nf[:], op=ALU.subtract), reads=[u.b, nf.b], writes=[u.b])

                def wrap(tt):
                    sc.op("dve", lambda e: e.tensor_scalar(out=nf[:], in0=tt[:], scalar1=0.5, scalar2=None, op0=ALU.is_ge), reads=[tt.b], writes=[nf.b])
                    sc.op("dve", lambda e: e.tensor_tensor(out=tt[:], in0=tt[:], in1=nf[:], op=ALU.subtract), reads=[tt.b, nf.b], writes=[tt.b])
                    sc.op("dve", lambda e: e.tensor_scalar(out=nf[:], in0=tt[:], scalar1=-0.5, scalar2=None, op0=ALU.is_lt), reads=[tt.b], writes=[nf.b])
                    sc.op("dve", lambda e: e.tensor_tensor(out=tt[:], in0=tt[:], in1=nf[:], op=ALU.add), reads=[tt.b, nf.b], writes=[tt.b])
                wrap(u)
                sc.op("act", lambda e: e.activation(out=sinT[:], in_=u[:], func=AF.Sin, scale=6.28318), reads=[u.b], writes=[sinT.b])
                sc.op("dve", lambda e: e.tensor_scalar(out=u[:], in0=u[:], scalar1=0.25, scalar2=None, op0=ALU.add), reads=[u.b, sinT.b], writes=[u.b])
                wrap(u)
                sc.op("act", lambda e: e.activation(out=cosT[:], in_=u[:], func=AF.Sin, scale=6.28318), reads=[u.b], writes=[cosT.b])
                sc.barrier()
            with ExitStack() as e2:
                wcq = TT("wcq", [128, KD, 384], BF16, scope=e2)
                wckv = TT("wckv", [128, KD, 256], BF16, scope=e2)
                wkpe = TT("wkpe", [128, KD, 32], BF16, scope=e2)
                wkps = TT("wkps", [128, KD, 32], BF16, scope=e2)
                qng = TT("qng", [128, 3], F32, scope=e2)
                kvng = TT("kvng", [128, 2], F32, scope=e2)
                wload(wcq, 2560, 384)
                wload(wckv, 2944, 256)
                wload(wkpe, 3200, 32)
                sc.dma("sp", qng[:], dd["qng"][:, l, :], writes=[qng.b])
                sc.dma("sp", kvng[:], dd["kvng"][:, l, :], writes=[kvng.b])
                sc.op("dve", lambda e: e.tensor_scalar(out=wkps[:, :, 0:16], in0=wkpe[:, :, 16:32], scalar1=-1.0, scalar2=None, op0=ALU.mult),
                      reads=[wkpe.b], writes=[wkps.b])
                sc.op("dve", lambda e: e.tensor_copy(out=wkps[:, :, 16:32], in_=wkpe[:, :, 0:16]), reads=[wkpe.b, wkps.b], writes=[wkps.b])
                junk = TT("junk", [128, 384], F32, scope=e2)
                cs_ = [TT(f"cqs{i}", [128, 384], F32, scope=e2) for i in range(2)]
                rs = [TT(f"crs{i}", [128, 1], F32, scope=e2) for i in range(2)]
                ci_ = 0
                for t in range(NT):
                    tsl = slice(t * 128, (t + 1) * 128)
                    for (wt, n, gn, dst, nk) in ((wcq, 384, qng, cqT, 3), (wckv, 256, kvng, ckvT, 2)):
                        ps = nextps()

                        def mmc(e, ps=ps, wt=wt, n=n, tsl=tsl):
                            ins = None
                            for k in range(KD):
                                ins = e.matmul(ps[:, 0:n], lhsT=hT[:, k, tsl], rhs=wt[:, k, :], start=(k == 0), stop=(k == KD - 1))
                            return ins
                        sc.op("pe", mmc, reads=[wt.b, hT.b], writes=[ps.b])
                        r = rs[ci_ % 2]
                        cq = cs_[ci_ % 2]
                        ci_ += 1
                        sc.op("act", lambda e, ps=ps, n=n, r=r: e.activation(out=junk[:, 0:n], in_=ps[:, 0:n], func=AF.Square, accum_out=r[:, 0:1]),
                              reads=[ps.b], writes=[junk.b, r.b])
                        sc.op("dve", lambda e, r=r, n=n: e.tensor_scalar(out=r[:], in0=r[:], scalar1=1.0 / n, scalar2=EPS, op0=ALU.mult, op1=ALU.add),
                              reads=[r.b], writes=[r.b])
                        sc.op("act", lambda e, r=r: e.activation(out=r[:], in_=r[:], func=AF.Sqrt), reads=[r.b], writes=[r.b])
                        sc.op("dve", lambda e, r=r: e.reciprocal(out=r[:], in_=r[:]), reads=[r.b], writes=[r.b])
                        sc.op("dve", lambda e, ps=ps, n=n, r=r, cq=cq: e.tensor_scalar(out=cq[:, 0:n], in0=ps[:, 0:n], scalar1=r[:, 0:1], scalar2=None, op0=ALU.mult),
                              reads=[ps.b, r.b], writes=[cq.b])
                        pt = nextps()

                        def trc(e, pt=pt, cq=cq, nk=nk):
                            ins = None
                            for j in range(nk):
                                ins = e.transpose(pt[:, j * 128:(j + 1) * 128], cq[:, j * 128:(j + 1) * 128], ident[:])
                            return ins
                        sc.op("pe", trc, reads=[cq.b, ident.b], writes=[pt.b])
                        sc.op("dve", lambda e, pt=pt, nk=nk, gn=gn, dst=dst, tsl=tsl: e.tensor_tensor(
                            out=dst[:, :, tsl], in0=pt[:, 0:nk * 128].rearrange("p (j c) -> p j c", c=128),
                            in1=gn[:].unsqueeze(2).to_broadcast([128, nk, 128]), op=ALU.mult), reads=[pt.b, gn.b], writes=[dst.b])
                t1 = TT("kt1", [128, 512], F32, scope=e2)
                t2 = TT("kt2", [128, 512], F32, scope=e2)
                R = slice(64, 96)
                for blk in range(NB):
                    ts = slice(blk * 512, (blk + 1) * 512)
                    ps = nextps()
                    ps2 = nextps()

                    def mmp(e, pp, wt, ts=ts):
                        ins = None
                        for k in range(KD):
                            ins = e.matmul(pp[R, :], lhsT=wt[:, k, :], rhs=hT[:, k, ts], start=(k == 0), stop=(k == KD - 1))
                        return ins
                    sc.op("pe", lambda e, ps=ps: mmp(e, ps, wkpe), reads=[wkpe.b, hT.b], writes=[ps.b])
                    sc.op("pe", lambda e, ps2=ps2: mmp(e, ps2, wkps), reads=[wkps.b, hT.b], writes=[ps2.b])
                    sc.op("dve", lambda e, ps=ps, ts=ts: e.tensor_tensor(out=t1[R, :], in0=ps[R, :], in1=cosT[R, ts], op=ALU.mult), reads=[ps.b, cosT.b], writes=[t1.b])
                    sc.op("dve", lambda e, ps2=ps2, ts=ts: e.tensor_tensor(out=t2[R, :], in0=ps2[R, :], in1=sinT[R, ts], op=ALU.mult), reads=[ps2.b, sinT.b], writes=[t2.b])
                    sc.op("dve", lambda e, ts=ts: e.tensor_tensor(out=kpeT[R, ts], in0=t1[R, :], in1=t2[R, :], op=ALU.add), reads=[t1.b, t2.b], writes=[kpeT.b])
                sc.barrier()
            with ExitStack() as e2:
                wqu = TT("wqu", [128, 3, 768], BF16, scope=e2)
                wqs = TT("wqs", [128, 3, 768], BF16, scope=e2)
                wkv = TT("wkv", [128, 2, 1024], BF16, scope=e2)
                sc.dma("pool", wqu[:], dd["w_qup"][l].rearrange("(k p) c -> p k c", p=128), writes=[wqu.b])
                sc.dma("pool", wkv[:], dd["w_kvup"][l].rearrange("(k p) c -> p k c", p=128), writes=[wkv.b])
                sc.op("dve", lambda e: e.tensor_copy(out=wqs[:], in_=wqu[:]), reads=[wqu.b], writes=[wqs.b])
                v4 = lambda tt: tt[:].rearrange("p k (h c) -> p k h c", c=96)
                sc.op("dve", lambda e: e.tensor_scalar(out=v4(wqs)[:, :, :, 64:80], in0=v4(wqu)[:, :, :, 80:96], scalar1=-1.0, scalar2=None, op0=ALU.mult),
                      reads=[wqu.b, wqs.b], writes=[wqs.b])
                sc.op("dve", lambda e: e.tensor_copy(out=v4(wqs)[:, :, :, 80:96], in_=v4(wqu)[:, :, :, 64:80]), reads=[wqu.b, wqs.b], writes=[wqs.b])
                vh = [TT(f"vh{i}", [128, NT, 64], BF16, scope=e2) for i in range(2)]
                ones64 = TT("ones64", [128, 64], BF16, scope=e2)
                sc.op("dve", lambda e: e.memset(ones64[:], 1.0), writes=[ones64.b])
                kfull = [TT(f"kfull{i}", [128, S], BF16, scope=e2) for i in range(2)]
                qrot = [TT(f"qrot{i}", [128, 512], BF16, scope=e2) for i in range(2)]
                pT = [TT(f"pT{i}", [128, 512], BF16, scope=e2) for i in range(4)]
                t1 = TT("qt1", [128, 512], F32, scope=e2)
                t2 = TT("qt2", [128, 512], F32, scope=e2)
                rden = TT("rden", [128, 512], F32, scope=e2)
                scale = 96.0 ** -0.5
                Q = slice(0, 96)
                pti = 0
                qi = 0
                for h in range(8):
                    kf = kfull[h % 2]
                    vv = vh[h % 2]
                    hp = slice((h % 2) * 64, (h % 2) * 64 + 64)
                    for blk in range(NB):
                        ts = slice(blk * 512, (blk + 1) * 512)
                        ps = nextps()

                        def mmkn(e, ps=ps, ts=ts, h=h):
                            ins = None
                            for k in range(2):
                                ins = e.matmul(ps[0:64, :], lhsT=wkv[:, k, h * 128:h * 128 + 64], rhs=ckvT[:, k, ts], start=(k == 0), stop=(k == 1))
                            return ins
                        sc.op("pe", mmkn, reads=[wkv.b, ckvT.b], writes=[ps.b])
                        sc.op("act", lambda e, ps=ps, ts=ts, kf=kf: e.copy(out=kf[0:64, ts], in_=ps[0:64, :]), reads=[ps.b], writes=[kf.b])
                    sc.op("pool", lambda e, kf=kf: e.tensor_copy(out=kf[64:96, :], in_=kpeT[64:96, :]), reads=[kpeT.b, kf.b], writes=[kf.b])
                    for t in range(NT):
                        tsl = slice(t * 128, (t + 1) * 128)
                        ps = nextps()

                        def mmvv(e, ps=ps, tsl=tsl, h=h):
                            ins = None
                            for k in range(2):
                                ins = e.matmul(ps[:, 0:64], lhsT=ckvT[:, k, tsl], rhs=wkv[:, k, h * 128 + 64:h * 128 + 128], start=(k == 0), stop=(k == 1))
                            return ins
                        sc.op("pe", mmvv, reads=[wkv.b, ckvT.b], writes=[ps.b])
                        sc.op("act", lambda e, ps=ps, t=t, vv=vv: e.copy(out=vv[:, t, :], in_=ps[:, 0:64]), reads=[ps.b], writes=[vv.b])
                    for blk in range(NB):
                        ts = slice(blk * 512, (blk + 1) * 512)
                        ps = PS[0]
                        ps2 = PS[1]

                        def mmqq(e, pp, wt, ts=ts, h=h):
                            ins = None
                            for k in range(3):
                                ins = e.matmul(pp[Q, :], lhsT=wt[:, k, h * 96:(h + 1) * 96], rhs=cqT[:, k, ts], start=(k == 0), stop=(k == 2))
                            return ins
                        sc.op("pe", lambda e, ps=ps: mmqq(e, ps, wqu), reads=[wqu.b, cqT.b], writes=[ps.b])
                        sc.op("pe", lambda e, ps2=ps2: mmqq(e, ps2, wqs), reads=[wqs.b, cqT.b], writes=[ps2.b])
                        qr = qrot[qi % 2]
                        qi += 1
                        sc.op("dve", lambda e, ps=ps, ts=ts: e.tensor_tensor(out=t1[Q, :], in0=ps[Q, :], in1=cosT[Q, ts], op=ALU.mult), reads=[ps.b, cosT.b], writes=[t1.b])
                        sc.op("dve", lambda e, ps2=ps2, ts=ts: e.tensor_tensor(out=t2[Q, :], in0=ps2[Q, :], in1=sinT[Q, ts], op=ALU.mult), reads=[ps2.b, sinT.b], writes=[t2.b])
                        sc.op("dve", lambda e, qr=qr: e.tensor_tensor(out=qr[Q, :], in0=t1[Q, :], in1=t2[Q, :], op=ALU.add), reads=[t1.b, t2.b], writes=[qr.b])
                        pn = PS[4 + (qi % 2)]
                        pd = PS[6 + (qi % 2)]
                        LA2 = 2
                        pbuf = {}
                        for step in range(NT + LA2):
                            if step < NT:
                                kt = step
                                ksl = slice(kt * 128, (kt + 1) * 128)
                                sps = PS[kt % 4]
                                p_ = pT[pti % 4]
                                pti += 1
                                pbuf[kt] = p_
                                sc.op("pe", lambda e, sps=sps, ksl=ksl, kf=kf, qr=qr: e.matmul(sps[:, :], lhsT=kf[Q, ksl], rhs=qr[Q, :], start=True, stop=True),
                                      reads=[kf.b, qr.b], writes=[sps.b])
                                sc.op("act", lambda e, sps=sps, p_=p_: e.activation(out=p_[:], in_=sps[:, :], func=AF.Exp, scale=scale), reads=[sps.b], writes=[p_.b])
                            if step >= LA2:
                                kt = step - LA2
                                p_ = pbuf[kt]

                                def mmpv(e, p_=p_, kt=kt, vv=vv, pn=pn, pd=pd, hp=hp):
                                    e.matmul(pn[hp, :], lhsT=vv[:, kt, :], rhs=p_[:], start=(kt == 0), stop=(kt == NT - 1))
                                    return e.matmul(pd[hp, :], lhsT=ones64[:], rhs=p_[:], start=(kt == 0), stop=(kt == NT - 1))
                                sc.op("pe", mmpv, reads=[p_.b, vv.b, ones64.b], writes=[pn.b, pd.b])
                        sc.op("dve", lambda e, pd=pd, hp=hp: e.reciprocal(out=rden[hp, :], in_=pd[hp, :]), reads=[pd.b], writes=[rden.b])
                        sc.op("dve", lambda e, pn=pn, hp=hp, h=h, ts=ts: e.tensor_tensor(out=mlaT[hp, h // 2, ts], in0=pn[hp, :], in1=rden[hp, :], op=ALU.mult),
                              reads=[pn.b, rden.b], writes=[mlaT.b])
                sc.barrier()
            sc.barrier()

        if dbg == "mla":
            sc.op("dve", lambda e: e.tensor_copy(out=xT[:, 0:4, :], in_=mlaT[:]), reads=[mlaT.b] + xb[0:4], writes=xb[0:4])
            sc.barrier()
            return
        with ExitStack() as ec:
            yT = TT("yT", [128, KD, S], BF16, scope=ec)
            woh = TT("woh", [128, 4, D], BF16, scope=ec)
            wom = TT("wom", [128, 4, D], BF16, scope=ec)
            sc.dma("pool", woh[:], dd["w_oh"][l].rearrange("(k p) c -> p k c", p=128), writes=[woh.b])
            sc.dma("pool", wom[:], dd["w_om"][l].rearrange("(k p) c -> p k c", p=128), writes=[wom.b])
            wga = [TT(f"wga{i}", [128, KD, 128], BF16, scope=ec) for i in range(2)]
            wgb = [TT(f"wgb{i}", [128, KD, 128], BF16, scope=ec) for i in range(2)]
            sga = [TT(f"sga{i}", [128, 512], F32, scope=ec) for i in range(2)]
            sgb = [TT(f"sgb{i}", [128, 512], F32, scope=ec) for i in range(2)]
            ci_ = 0
            for m in range(KD):
                wa_ = wga[m % 2]
                wb_ = wgb[m % 2]
                wload(wa_, 3232 + m * 128, 128)
                wload(wb_, 3232 + 1024 + m * 128, 128)
                for blk in range(NB):
                    ts = slice(blk * 512, (blk + 1) * 512)
                    pya, pyb, pga, pgb = nextps(), nextps(), nextps(), nextps()

                    def mm4(e, pp, wt, src, nk, ts=ts, m=m, whole=False):
                        ins = None
                        for k in range(nk):
                            lhs = wt[:, k, :] if whole else wt[:, k, m * 128:(m + 1) * 128]
                            ins = e.matmul(pp[:, :], lhsT=lhs, rhs=src[:, k, ts], start=(k == 0), stop=(k == nk - 1))
                        return ins
                    sc.op("pe", lambda e, pp=pya: mm4(e, pp, woh, ogT, 4), reads=[woh.b, ogT.b], writes=[pya.b])
                    sc.op("pe", lambda e, pp=pyb: mm4(e, pp, wom, mlaT, 4), reads=[wom.b, mlaT.b], writes=[pyb.b])
                    sc.op("pe", lambda e, pp=pga, wa_=wa_: mm4(e, pp, wa_, hT, KD, whole=True), reads=[wa_.b, hT.b], writes=[pga.b])
                    sc.op("pe", lambda e, pp=pgb, wb_=wb_: mm4(e, pp, wb_, hT, KD, whole=True), reads=[wb_.b, hT.b], writes=[pgb.b])
                    a_ = sga[ci_ % 2]
                    b_ = sgb[ci_ % 2]
                    ci_ += 1
                    sc.op("act", lambda e, a_=a_, pga=pga: e.activation(out=a_[:], in_=pga[:, :], func=AF.Sigmoid), reads=[pga.b], writes=[a_.b])
                    sc.op("act", lambda e, b_=b_, pgb=pgb: e.activation(out=b_[:], in_=pgb[:, :], func=AF.Sigmoid), reads=[pgb.b], writes=[b_.b])
                    sc.op("dve", lambda e, a_=a_, pya=pya: e.tensor_tensor(out=a_[:], in0=pya[:, :], in1=a_[:], op=ALU.mult), reads=[pya.b, a_.b], writes=[a_.b])
                    sc.op("dve", lambda e, b_=b_, pyb=pyb: e.tensor_tensor(out=b_[:], in0=pyb[:, :], in1=b_[:], op=ALU.mult), reads=[pyb.b, b_.b], writes=[b_.b])
                    sc.op("pool", lambda e, a_=a_, b_=b_, m=m, ts=ts: e.tensor_tensor(out=yT[:, m, ts], in0=a_[:], in1=b_[:], op=ALU.add),
                          reads=[a_.b, b_.b], writes=[yT.b])
            wo = [TT(f"wo{i}", [128, KD, 128], BF16, scope=ec) for i in range(2)]
            for m in range(KD):
                w_ = wo[m % 2]
                sc.dma("pool", w_[:], dd["w_o"][l, :, m * 128:(m + 1) * 128].rearrange("(k p) c -> p k c", p=128), writes=[w_.b])
                for blk in range(NB):
                    ts = slice(blk * 512, (blk + 1) * 512)
                    ps = nextps()

                    def mmo2(e, ps=ps, w_=w_, ts=ts):
                        ins = None
                        for k in range(KD):
                            ins = e.matmul(ps[:, :], lhsT=w_[:, k, :], rhs=yT[:, k, ts], start=(k == 0), stop=(k == KD - 1))
                        return ins
                    sc.op("pe", mmo2, reads=[w_.b, yT.b], writes=[ps.b])
                    sc.op("dve", lambda e, ps=ps, m=m, ts=ts: e.scalar_tensor_tensor(
                        out=xT[:, m, ts], in0=ps[:, :], scalar=modT[:, l, 16 + m:16 + m + 1], in1=xT[:, m, ts],
                        op0=ALU.mult, op1=ALU.add), reads=[ps.b, modT.b, xb[m]], writes=[xb[m]])
            sc.barrier()
        sc.barrier()


def build_program(L, do_mixer=True, do_moe=True, do_final=True, dbg=None):
    nc = bass.Bass("TRN2", target_bir_lowering=False)

    def din(name, shape, dt=F32):
        return nc.dram_tensor(name, list(shape), dt, kind="ExternalInput").ap()

    x_d = din("x", [S, D])
    ccol_d = din("ccol", [128, KD])
    pos_d = din("posrep", [128, S], I32)
    w_ada_d = din("w_ada", [L, D, 6 * D])
    bada_d = din("bada_col", [128, L, 48])
    nmg_d = din("nmg_col", [128, L, KD])
    nfg_d = din("nfg_col", [128, L, KD])
    fng_d = din("fng_col", [128, KD])
    w_in_d = din("w_in", [L, D, INW])
    lbl_col_d = din("lbl_col", [128, 4, 8])
    lbl_row_d = din("lbl_row", [128, 4, 1024])
    hgain_d = din("hgain_row", [128, L, 128])
    w_oh_d = din("w_out_hgrn", [L, 512, D])
    qng_d = din("qng_col", [128, L, 3])
    w_qup_d = din("w_q_up", [L, 384, 768])
    kvng_d = din("kvng_col", [128, L, 2])
    w_kvup_d = din("w_kv_up", [L, 256, 1024])
    w_om_d = din("w_out_mla", [L, 512, D])
    w_o_d = din("w_o", [L, D, D])
    w_r_d = din("w_router", [L, D, E])
    br_d = din("br_row", [128, L, E])
    w_gu_d = din("w_gate_up", [L, E, D, 2 * D])
    bg_d = din("bg_col", [L, 128, E * 8])
    bu_d = din("bu_col", [L, 128, E * 8])
    w_dn_d = din("w_down", [L, E, D, D])
    bd_d = din("b_down", [L, E, D])
    ident_d = din("ident", [128, 128])
    uext_d = din("uext", [128, 2, 67])
    mask_d = din("maskd", [128, 2, 64])
    freq_d = din("freqcol", [128, 1])
    y_d = nc.dram_tensor("y", [S, D], F32, kind="ExternalOutput").ap()

    with ExitStack() as es:
        sc = Sched(nc, es)

        def TT(name, shape, dt, scope=es, psum=False):
            return T(sc, scope, name, shape, dt, psum=psum)

        xT = TT("xT", [128, KD, S], F32)
        xb = [sc.buf(f"xTb{k}") for k in range(KD)]
        hT = TT("hT", [128, KD, S], BF16)
        ident = TT("ident_sb", [128, 128], F32)
        ones_col = TT("ones_col", [128, 1], F32)
        ones_row = TT("ones_row", [1, 128], F32)
        modT = TT("modT", [128, L, 48], F32)
        gsc1 = TT("gsc1", [128, L, KD], F32)
        gsc2 = TT("gsc2", [128, L, KD], F32)
        ccol = TT("ccol_sb", [128, KD], F32)
        PS = [TT(f"ps{i}", [128, 512], F32, psum=True) for i in range(8)]
        psi = [0]

        def nextps():
            p = PS[psi[0] % 8]
            psi[0] += 1
            return p

        sc.dma("sp", ident[:], ident_d, writes=[ident.b])
        sc.op("dve", lambda e: e.memset(ones_col[:], 1.0), writes=[ones_col.b])
        sc.op("dve", lambda e: e.memset(ones_row[:], 1.0), writes=[ones_row.b])
        sc.dma("sp", ccol[:], ccol_d, writes=[ccol.b])
        sc.op("act", lambda e: e.activation(out=ccol[:], in_=ccol[:], func=AF.Silu), reads=[ccol.b], writes=[ccol.b])

        with ExitStack() as es0:
            wa = [TT(f"wada{i}", [128, KD, 768], F32, scope=es0) for i in range(2)]
            bada = TT("bada_sb", [128, L, 48], F32, scope=es0)
            nmg = TT("nmg_sb", [128, L, KD], F32, scope=es0)
            nfg = TT("nfg_sb", [128, L, KD], F32, scope=es0)
            sc.dma("sp", bada[:], bada_d, writes=[bada.b])
            sc.dma("sp", nmg[:], nmg_d, writes=[nmg.b])
            sc.dma("sp", nfg[:], nfg_d, writes=[nfg.b])
            gi = 0
            for l in range(L):
                for grp in range(8):
                    w = wa[gi % 2]
                    gi += 1
                    sc.dma("sp", w[:], w_ada_d[l, :, grp * 768:(grp + 1) * 768].rearrange("(k p) c -> p k c", p=128),
                           writes=[w.b])
                    ps = nextps()

                    def mm(e, w=w, ps=ps):
                        ins = None
                        for j in range(6):
                            for k in range(KD):
                                ins = e.matmul(ps[:, j:j + 1], lhsT=w[:, k, j * 128:(j + 1) * 128], rhs=ccol[:, k:k + 1],
                                               start=(k == 0), stop=(k == KD - 1))
                        return ins
                    sc.op("pe", mm, reads=[w.b, ccol.b], writes=[ps.b])
                    sc.op("dve", lambda e, ps=ps, l=l, grp=grp: e.tensor_tensor(
                        out=modT[:, l, grp * 6:(grp + 1) * 6], in0=ps[:, 0:6], in1=bada[:, l, grp * 6:(grp + 1) * 6], op=ALU.add),
                        reads=[ps.b, bada.b], writes=[modT.b])
            for l in range(L):
                sc.op("dve", lambda e, l=l: e.scalar_tensor_tensor(out=gsc1[:, l, :], in0=modT[:, l, 8:16], scalar=1.0, in1=nmg[:, l, :],
                                                                   op0=ALU.add, op1=ALU.mult), reads=[modT.b, nmg.b], writes=[gsc1.b])
                sc.op("dve", lambda e, l=l: e.scalar_tensor_tensor(out=gsc2[:, l, :], in0=modT[:, l, 32:40], scalar=1.0, in1=nfg[:, l, :],
                                                                   op0=ALU.add, op1=ALU.mult), reads=[modT.b, nfg.b], writes=[gsc2.b])
            sc.barrier()

        with ExitStack() as es0:
            xin = [TT(f"xin{i}", [128, D], F32, scope=es0) for i in range(2)]
            for t in range(NT):
                xi = xin[t % 2]
                sc.dma("sp", xi[:], x_d[t * 128:(t + 1) * 128, :], writes=[xi.b])
                for half in range(2):
                    ps = nextps()

                    def tr(e, xi=xi, ps=ps, half=half):
                        ins = None
                        for j in range(4):
                            k = half * 4 + j
                            ins = e.transpose(ps[:, j * 128:(j + 1) * 128], xi[:, k * 128:(k + 1) * 128], ident[:])
                        return ins
                    sc.op("pe", tr, reads=[xi.b, ident.b], writes=[ps.b])
                    wr_ = xb[half * 4:(half + 1) * 4]
                    if half == 0:
                        sc.op("dve", lambda e, ps=ps, half=half, t=t: e.tensor_copy(
                            out=xT[:, half * 4:(half + 1) * 4, t * 128:(t + 1) * 128],
                            in_=ps[:, :].rearrange("p (j c) -> p j c", c=128)), reads=[ps.b], writes=wr_)
                    else:
                        sc.op("act", lambda e, ps=ps, half=half, t=t: e.copy(
                            out=xT[:, half * 4:(half + 1) * 4, t * 128:(t + 1) * 128],
                            in_=ps[:, :].rearrange("p (j c) -> p j c", c=128)), reads=[ps.b], writes=wr_)
            sc.barrier()

        def rstd_block(ps, rrow, ts, sq, cntbox):
            for k in range(KD):
                q = sq[cntbox[0] % 2]
                cntbox[0] += 1
                sc.op("act", lambda e, q=q, k=k: e.activation(out=q[:], in_=xT[:, k, ts], func=AF.Square),
                      reads=[xb[k]], writes=[q.b])
                sc.op("pe", lambda e, q=q, k=k: e.matmul(ps[0:1, :], lhsT=ones_col[:], rhs=q[:],
                                                         start=(k == 0), stop=(k == KD - 1)),
                      reads=[q.b, ones_col.b], writes=[ps.b])
            sc.op("dve", lambda e: e.tensor_scalar(out=rrow[0:1, ts], in0=ps[0:1, :], scalar1=1.0 / D, scalar2=EPS,
                                                   op0=ALU.mult, op1=ALU.add), reads=[ps.b], writes=[rrow.b])
            sc.op("act", lambda e: e.activation(out=rrow[0:1, ts], in_=rrow[0:1, ts], func=AF.Sqrt),
                  reads=[rrow.b], writes=[rrow.b])
            sc.op("dve", lambda e: e.reciprocal(out=rrow[0:1, ts], in_=rrow[0:1, ts]), reads=[rrow.b], writes=[rrow.b])

        def norm_modulate(l, gsc, sh_off, h32_cb=None):
            with ExitStack() as es1:
                sq = [TT(f"sq{i}", [128, 512], F32, scope=es1) for i in range(2)]
                rrow = TT("rrow", [1, S], F32, scope=es1)
                tmp = [TT(f"ntmp{i}", [128, 512], F32, scope=es1) for i in range(2)]
                h32 = TT("h32", [128, KD, 512], F32, scope=es1) if h32_cb is not None else None
                cntbox = [0]
                for blk in range(NB):
                    ts = slice(blk * 512, (blk + 1) * 512)
                    ps = nextps()
                    rstd_block(ps, rrow, ts, sq, cntbox)
                    pb = nextps()
                    sc.op("pe", lambda e, pb=pb, ts=ts: e.matmul(pb[:, :], lhsT=ones_row[:], rhs=rrow[0:1, ts], start=True, stop=True),
                          reads=[rrow.b, ones_row.b], writes=[pb.b])
                    for k in range(KD):
                        tm = tmp[cntbox[0] % 2]
                        cntbox[0] += 1
                        sc.op("dve", lambda e, tm=tm, k=k, ts=ts, pb=pb: e.tensor_tensor(out=tm[:], in0=xT[:, k, ts], in1=pb[:, :], op=ALU.mult),
                              reads=[xb[k], pb.b], writes=[tm.b])
                        if h32 is None:
                            sc.op("act", lambda e, tm=tm, k=k, ts=ts: e.activation(
                                out=hT[:, k, ts], in_=tm[:], func=AF.Identity, scale=gsc[:, l, k:k + 1],
                                bias=modT[:, l, sh_off + k:sh_off + k + 1]),
                                reads=[tm.b, gsc.b, modT.b], writes=[hT.b])
                        else:
                            sc.op("act", lambda e, tm=tm, k=k: e.activation(
                                out=h32[:, k, :], in_=tm[:], func=AF.Identity, scale=gsc[:, l, k:k + 1],
                                bias=modT[:, l, sh_off + k:sh_off + k + 1]),
                                reads=[tm.b, gsc.b, modT.b], writes=[h32.b])
                    if h32 is not None:
                        sc.op("pool", lambda e, ts=ts: e.tensor_copy(out=hT[:, :, ts], in_=h32[:, :, :]), reads=[h32.b], writes=[hT.b])
                        h32_cb(blk, h32)
                sc.barrier()

        def moe_layer(l):
            g2off = 40
            with ExitStack() as es1:
                wr = TT("wr_sb", [128, KD, E], F32, scope=es1)
                brr = TT("br_sb", [128, E], F32, scope=es1)
                wT = TT("wT_sb", [E, S], F32, scope=es1)
                bdn = TT("bdn_sb", [E, D], F32, scope=es1)
                bg = TT("bg_sb", [128, E * 8], F32, scope=es1)
                bu = TT("bu_sb", [128, E * 8], F32, scope=es1)
                sc.dma("sp", wr[:], w_r_d[l].rearrange("(k p) e -> p k e", p=128), writes=[wr.b])
                sc.dma("sp", brr[:], br_d[:, l, :], writes=[brr.b])
                sc.dma("sp", bdn[:], bd_d[l], writes=[bdn.b])
                sc.dma("sp", bg[:], bg_d[l], writes=[bg.b])
                sc.dma("sp", bu[:], bu_d[l], writes=[bu.b])
                sc.op("dve", lambda e: e.tensor_scalar(out=bu[:], in0=bu[:], scalar1=1.0, scalar2=None, op0=ALU.add),
                      reads=[bu.b], writes=[bu.b])
                lg = TT("lg", [128, E], F32, scope=es1)
                top8 = TT("top8", [128, 8], F32, scope=es1)
                negm = TT("negm", [128, 1], F32, scope=es1)
                ex = TT("ex", [128, E], F32, scope=es1)
                ssum = TT("ssum", [128, 1], F32, scope=es1)
                wtm = TT("wtm", [128, E], F32, scope=es1)

                def router(blk, h32):
                    for j in range(4):
                        t = blk * 4 + j
                        ps = nextps()

                        def mm(e, ps=ps, j=j):
                            ins = None
                            for k in range(KD):
                                ins = e.matmul(ps[:, 0:E], lhsT=h32[:, k, j * 128:(j + 1) * 128], rhs=wr[:, k, :],
                                               start=(k == 0), stop=(k == KD - 1))
                            return ins
                        sc.op("pe", mm, reads=[h32.b, wr.b], writes=[ps.b])
                        sc.op("dve", lambda e, ps=ps: e.tensor_tensor(out=lg[:], in0=ps[:, 0:E], in1=brr[:], op=ALU.add),
                              reads=[ps.b, brr.b], writes=[lg.b])
                        sc.op("dve", lambda e: e.max(out=top8[:], in_=lg[:]), reads=[lg.b], writes=[top8.b])
                        sc.op("dve", lambda e: e.tensor_scalar(out=negm[:], in0=top8[:, 0:1], scalar1=-1.0, scalar2=None, op0=ALU.mult),
                              reads=[top8.b], writes=[negm.b])
                        sc.op("act", lambda e: e.activation(out=ex[:], in_=lg[:], func=AF.Exp, bias=negm[:, 0:1], scale=1.0),
                              reads=[lg.b, negm.b], writes=[ex.b])
                        sc.op("dve", lambda e: e.tensor_scalar(out=lg[:], in0=lg[:], scalar1=top8[:, 3:4], scalar2=None, op0=ALU.is_ge),
                              reads=[lg.b, top8.b, ex.b], writes=[lg.b])
                        sc.op("dve", lambda e: e.tensor_tensor(out=wtm[:], in0=ex[:], in1=lg[:], op=ALU.mult),
                              reads=[ex.b, lg.b], writes=[wtm.b])
                        sc.op("dve", lambda e: e.reduce_sum(out=ssum[:], in_=wtm[:], axis=AX.X), reads=[wtm.b], writes=[ssum.b])
                        sc.op("dve", lambda e: e.reciprocal(out=ssum[:], in_=ssum[:]), reads=[ssum.b], writes=[ssum.b])
                        sc.op("dve", lambda e: e.tensor_scalar(out=wtm[:], in0=wtm[:], scalar1=ssum[:, 0:1], scalar2=None, op0=ALU.mult),
                              reads=[wtm.b, ssum.b], writes=[wtm.b])
                        pt = nextps()
                        sc.op("pe", lambda e, pt=pt: e.transpose(pt[0:E, 0:128], wtm[:], ident[:]), reads=[wtm.b, ident.b], writes=[pt.b])
                        sc.op("act", lambda e, pt=pt, t=t: e.copy(out=wT[:, t * 128:(t + 1) * 128], in_=pt[0:E, 0:128]),
                              reads=[pt.b], writes=[wT.b])

                norm_modulate(l, gsc2, 24, h32_cb=router)

                for blk in range(NB):
                    ts = slice(blk * 512, (blk + 1) * 512)
                    for m in range(KD):
                        ps = nextps()
                        sc.op("pe", lambda e, ps=ps, m=m, ts=ts: e.matmul(ps[:, :], lhsT=bdn[:, m * 128:(m + 1) * 128], rhs=wT[:, ts],
                                                                          start=True, stop=True), reads=[bdn.b, wT.b], writes=[ps.b])
                        sc.op("dve", lambda e, ps=ps, m=m, ts=ts: e.scalar_tensor_tensor(
                            out=xT[:, m, ts], in0=ps[:, :], scalar=modT[:, l, g2off + m:g2off + m + 1], in1=xT[:, m, ts],
                            op0=ALU.mult, op1=ALU.add), reads=[ps.b, modT.b, xb[m]], writes=[xb[m]])

                NU = 2
                wgu = [TT(f"wgu{i}", [128, KD, 1024], BF16, scope=es1) for i in range(NU)]
                wdn = [TT(f"wdn{i}", [128, 4, D], BF16, scope=es1) for i in range(NU)]
                act = TT("actb", [128, 4, S], BF16, scope=es1)
                actb = [sc.buf(f"actblk{i}") for i in range(NB)]
                wbc = [TT(f"wbc{i}", [128, S], BF16, scope=es1) for i in range(2)]
                gt = [TT(f"gt{i}", [128, 512], F32, scope=es1) for i in range(2)]
                ut = [TT(f"ut{i}", [128, 512], F32, scope=es1) for i in range(2)]
                st = [TT(f"st{i}", [128, 512], F32, scope=es1) for i in range(2)]
                pc = 0
                NUNITS = E * 2

                def load_unit(u):
                    e_, hf = divmod(u, 2)
                    wg = wgu[u % NU]
                    wd = wdn[u % NU]
                    sc.dma("pool", wg[:], w_gu_d[l, e_, :, hf * 1024:(hf + 1) * 1024].rearrange("(k p) c -> p k c", p=128),
                           writes=[wg.b])
                    sc.dma("pool", wd[:], w_dn_d[l, e_, hf * 512:(hf + 1) * 512, :].rearrange("(c p) n -> p c n", p=128),
                           writes=[wd.b])

                load_unit(0)
                load_unit(1)
                for e_ in range(E):
                    wb = wbc[e_ % 2]
                    for blk in range(NB):
                        ts = slice(blk * 512, (blk + 1) * 512)
                        ps = nextps()
                        sc.op("pe", lambda e, ps=ps, ts=ts, e_=e_: e.matmul(
                            ps[:, :], lhsT=ident[0:E, e_:e_ + 1].to_broadcast([E, 128]), rhs=wT[:, ts],
                            start=True, stop=True), reads=[ident.b, wT.b], writes=[ps.b])
                        sc.op("act", lambda e, ps=ps, ts=ts, wb=wb: e.copy(out=wb[:, ts], in_=ps[:, :]), reads=[ps.b], writes=[wb.b])
                    for hf in range(2):
                        u = e_ * 2 + hf
                        wg = wgu[u % NU]
                        wd = wdn[u % NU]
                        pending = []
                        for blk in range(NB):
                            ts = slice(blk * 512, (blk + 1) * 512)
                            for c in range(4):
                                fc = hf * 4 + c
                                bcol = e_ * 8 + fc
                                pg = nextps()
                                pu = nextps()

                                def mmg(e, pp, off, wg=wg, c=c, ts=ts):
                                    ins = None
                                    for k in range(KD):
                                        ins = e.matmul(pp[:, :], lhsT=wg[:, k, c * 256 + off:c * 256 + 256:2], rhs=hT[:, k, ts],
                                                       start=(k == 0), stop=(k == KD - 1))
                                    return ins
                                sc.op("pe", lambda e, pg=pg: mmg(e, pg, 0), reads=[wg.b, hT.b], writes=[pg.b])
                                sc.op("pe", lambda e, pu=pu: mmg(e, pu, 1), reads=[wg.b, hT.b], writes=[pu.b])
                                g = gt[pc % 2]
                                uu = ut[pc % 2]
                                s_ = st[pc % 2]
                                pc += 1
                                sc.op("dve", lambda e, g=g, pg=pg, bcol=bcol: e.tensor_scalar(
                                    out=g[:], in0=pg[:, :], scalar1=bg[:, bcol:bcol + 1], scalar2=7.0, op0=ALU.add, op1=ALU.min),
                                    reads=[pg.b, bg.b], writes=[g.b])
                                for fn in pending:
                                    fn()
                                pending = []
                                sc.op("dve", lambda e, uu=uu, pu=pu, bcol=bcol: e.tensor_scalar(
                                    out=uu[:], in0=pu[:, :], scalar1=bu[:, bcol:bcol + 1], scalar2=-6.0, op0=ALU.add, op1=ALU.max),
                                    reads=[pu.b, bu.b], writes=[uu.b])
                                sc.op("act", lambda e, s_=s_, g=g: e.activation(out=s_[:], in_=g[:], func=AF.Sigmoid, scale=1.702),
                                      reads=[g.b], writes=[s_.b])
                                sc.op("pool", lambda e, s_=s_, g=g: e.tensor_tensor(out=s_[:], in0=s_[:], in1=g[:], op=ALU.mult),
                                      reads=[s_.b, g.b], writes=[s_.b])
                                sc.op("pool", lambda e, s_=s_, wb=wb, ts=ts: e.tensor_tensor(out=s_[:], in0=s_[:], in1=wb[:, ts], op=ALU.mult),
                                      reads=[s_.b, wb.b], writes=[s_.b])

                                def fin(s_=s_, uu=uu, c=c, ts=ts, blk=blk):
                                    sc.op("dve", lambda e: e.scalar_tensor_tensor(
                                        out=act[:, c, ts], in0=uu[:], scalar=8.0, in1=s_[:], op0=ALU.min, op1=ALU.mult),
                                        reads=[s_.b, uu.b], writes=[actb[blk]])
                                pending.append(fin)
                        for fn in pending:
                            fn()
                        pending = []
                        for blk in range(NB):
                            ts = slice(blk * 512, (blk + 1) * 512)
                            for m in range(KD):
                                po = nextps()

                                def mmd(e, po=po, wd=wd, m=m, ts=ts):
                                    ins = None
                                    for c in range(4):
                                        ins = e.matmul(po[:, :], lhsT=wd[:, c, m * 128:(m + 1) * 128], rhs=act[:, c, ts],
                                                       start=(c == 0), stop=(c == 3))
                                    return ins
                                sc.op("pe", mmd, reads=[wd.b, actb[blk]], writes=[po.b])
                                sc.op("dve", lambda e, po=po, m=m, ts=ts: e.scalar_tensor_tensor(
                                    out=xT[:, m, ts], in0=po[:, :], scalar=modT[:, l, g2off + m:g2off + m + 1], in1=xT[:, m, ts],
                                    op0=ALU.mult, op1=ALU.add), reads=[po.b, modT.b, xb[m]], writes=[xb[m]])
                        if u + 2 < NUNITS:
                            load_unit(u + 2)
                sc.barrier()

        for l in range(L):
            if do_mixer:
                mixer_layer(nc, sc, TT, nextps, PS, l, xT, xb, hT, ident, modT, gsc1, norm_modulate,
                            dict(w_in=w_in_d, lbl_col=lbl_col_d, lbl_row=lbl_row_d, hgain=hgain_d, w_oh=w_oh_d, qng=qng_d,
                                 w_qup=w_qup_d, kvng=kvng_d, w_kvup=w_kvup_d, w_om=w_om_d, w_o=w_o_d, uext=uext_d, mask=mask_d,
                                 freq=freq_d, pos=pos_d), dbg)
            if do_moe:
                moe_layer(l)

        with ExitStack() as es1:
            fng = TT("fng_sb", [128, KD], F32, scope=es1)
            sc.dma("sp", fng[:], fng_d, writes=[fng.b])
            rrow = TT("rrowf", [1, S], F32, scope=es1)
            sq = [TT(f"sqf{i}", [128, 512], F32, scope=es1) for i in range(2)]
            xn = TT("xn", [128, KD, 512], F32, scope=es1)
            yo = [TT(f"yo{i}", [128, D], F32, scope=es1) for i in range(2)]
            ybufs = [sc.buf("yd0"), sc.buf("yd1")]
            cntbox = [0]
            for blk in range(NB):
                ts = slice(blk * 512, (blk + 1) * 512)
                if do_final:
                    ps = nextps()
                    rstd_block(ps, rrow, ts, sq, cntbox)
                    pb = nextps()
                    sc.op("pe", lambda e, pb=pb, ts=ts: e.matmul(pb[:, :], lhsT=ones_row[:], rhs=rrow[0:1, ts], start=True, stop=True),
                          reads=[rrow.b, ones_row.b], writes=[pb.b])
                    for k in range(KD):
                        sc.op("dve", lambda e, k=k, ts=ts, pb=pb: e.scalar_tensor_tensor(
                            out=xn[:, k, :], in0=xT[:, k, ts], scalar=fng[:, k:k + 1], in1=pb[:, :], op0=ALU.mult, op1=ALU.mult),
                            reads=[xb[k], pb.b, fng.b], writes=[xn.b])
                else:
                    sc.op("dve", lambda e, ts=ts: e.tensor_copy(out=xn[:, :, :], in_=xT[:, :, ts]), reads=xb, writes=[xn.b])
                for j in range(4):
                    t = blk * 4 + j
                    y = yo[t % 2]
                    for half in range(2):
                        ps = nextps()

                        def tr(e, ps=ps, half=half, j=j):
                            ins = None
                            for jj in range(4):
                                k = half * 4 + jj
                                ins = e.transpose(ps[:, jj * 128:(jj + 1) * 128], xn[:, k, j * 128:(j + 1) * 128], ident[:])
                            return ins
                        sc.op("pe", tr, reads=[xn.b, ident.b], writes=[ps.b])
                        if half == 0:
                            sc.op("dve", lambda e, ps=ps, y=y, half=half: e.tensor_copy(out=y[:, half * 512:(half + 1) * 512], in_=ps[:, :]),
                                  reads=[ps.b], writes=[y.b])
                        else:
                            sc.op("act", lambda e, ps=ps, y=y, half=half: e.copy(out=y[:, half * 512:(half + 1) * 512], in_=ps[:, :]),
                                  reads=[ps.b], writes=[y.b])
                    sc.dma("sp", y_d[t * 128:(t + 1) * 128, :], y[:], reads=[y.b], writes=[ybufs[t % 2]])
            sc.barrier()
    return nc


def _consts():
    s = np.arange(64)
    uext = np.zeros((2, 64, 67), np.float32)
    uext[0, :, :64] = (s[:, None] <= s[None, :]).astype(np.float32) - (s[:, None] <= 31).astype(np.float32)
    uext[0, :, 64] = (s <= 31)
    uext[0, :, 65] = 1.0
    uext[0, :, 66] = (s > 31)
    uext[1, :, :64] = (s[:, None] >= s[None, :]).astype(np.float32) - (s[:, None] >= 32).astype(np.float32)
    uext[1, :, 64] = (s >= 32)
    uext[1, :, 65] = 1.0
    uext[1, :, 66] = (s < 32)
    uext = np.concatenate([uext, uext], axis=1)
    mask = np.zeros((2, 64, 64), np.float32)
    mask[0] = (s[:, None] <= s[None, :])
    mask[1] = (s[:, None] >= s[None, :])
    mask = np.concatenate([mask, mask], axis=1)
    freq = np.zeros((128, 1), np.float32)
    inv = (10000.0 ** (-np.arange(0, 32, 2, dtype=np.float32) / 32.0)).astype(np.float32)
    freq[64:80, 0] = inv / np.float32(2 * np.pi)
    freq[80:96, 0] = inv / np.float32(2 * np.pi)
    return dict(ident=np.eye(128, dtype=np.float32),
                uext=np.ascontiguousarray(uext.transpose(1, 0, 2)),
                maskd=np.ascontiguousarray(mask.transpose(1, 0, 2)),
                freqcol=freq)


def _prep(inputs, L, b):
    f = lambda a: np.ascontiguousarray(np.asarray(a, dtype=np.float32))
    col = lambda v, n: np.ascontiguousarray(v.reshape(v.shape[0], n, 128).transpose(2, 0, 1))
    m = {}
    m["x"] = f(inputs["x"][b])
    m["ccol"] = np.ascontiguousarray(f(inputs["c"][b]).reshape(KD, 128).T)
    m["posrep"] = np.ascontiguousarray(np.broadcast_to(np.asarray(inputs["positions"][b], dtype=np.int32)[None, :], (128, S)))
    m["w_ada"] = f(inputs["w_ada"][:L])
    m["bada_col"] = col(f(inputs["b_ada"][:L]), 48)
    m["nmg_col"] = col(f(inputs["norm_mix_gain"][:L]), KD)
    m["nfg_col"] = col(f(inputs["norm_ffn_gain"][:L]), KD)
    m["fng_col"] = np.ascontiguousarray(f(inputs["final_norm_gain"]).reshape(KD, 128).T)
    m["w_in"] = f(inputs["w_in"][:L])
    lbl = f(inputs["hgrn_lb_logits"])
    m["lbl_col"] = np.ascontiguousarray(lbl.reshape(4, 8, 128).transpose(2, 0, 1))
    m["lbl_row"] = np.ascontiguousarray(np.broadcast_to(lbl.reshape(1, 4, 1024), (128, 4, 1024)))
    m["hgain_row"] = np.ascontiguousarray(np.broadcast_to(f(inputs["hgrn_out_norm_gain"][:L])[None], (128, L, 128)))
    m["w_out_hgrn"] = f(inputs["w_out_hgrn"][:L])
    m["qng_col"] = col(f(inputs["mla_q_norm_gain"][:L]), 3)
    m["w_q_up"] = f(inputs["w_q_up"][:L])
    m["kvng_col"] = col(f(inputs["mla_kv_norm_gain"][:L]), 2)
    m["w_kv_up"] = f(inputs["w_kv_up"][:L])
    m["w_out_mla"] = f(inputs["w_out_mla"][:L])
    m["w_o"] = f(inputs["w_o"][:L])
    m["w_router"] = f(inputs["w_router"][:L])
    m["br_row"] = np.ascontiguousarray(np.broadcast_to(f(inputs["b_router"][:L])[None], (128, L, E)))
    m["w_gate_up"] = f(inputs["w_gate_up"][:L])
    bgu = f(inputs["b_gate_up"][:L])
    bgc = bgu[:, :, 0::2].reshape(L, E, 8, 128).transpose(0, 3, 1, 2).reshape(L, 128, E * 8)
    buc = bgu[:, :, 1::2].reshape(L, E, 8, 128).transpose(0, 3, 1, 2).reshape(L, 128, E * 8)
    m["bg_col"] = np.ascontiguousarray(bgc)
    m["bu_col"] = np.ascontiguousarray(buc)
    m["w_down"] = f(inputs["w_down"][:L])
    m["b_down"] = f(inputs["b_down"][:L])
    return m


_PROG = {}


def kernel(**inputs):
    L = 4
    nb = 8
    if "full" not in _PROG:
        _PROG["full"] = build_program(L)
    nc = _PROG["full"]
    consts = _consts()
    shared = None
    in_maps = []
    for b in range(nb):
        m = _prep(inputs, L, b)
        if shared is None:
            shared = {k: v for k, v in m.items() if k not in ("x", "ccol", "posrep")}
        else:
            for k in shared:
                m[k] = shared[k]
        m.update(consts)
        in_maps.append(m)
    res = run_bass_kernel_spmd(nc, in_maps, core_ids=list(range(nb)))
    out = np.stack([np.asarray(r["y"], dtype=np.float32) for r in res.results], axis=0)
    return out
```

```python
import numpy as np
import os
from contextlib import ExitStack
import concourse.bass as bass
import concourse.mybir as mybir
from concourse.bass_utils import run_bass_kernel_spmd

F32 = mybir.dt.float32
BF16 = mybir.dt.bfloat16
I32 = mybir.dt.int32
AF = mybir.ActivationFunctionType
ALU = mybir.AluOpType
AX = mybir.AxisListType

S = 2048
D = 1024
NT = 16
NB = 4
KD = 8
E = 32
INW = 5280
EPS = 1e-6


class Buf:
    __slots__ = ("name", "writer", "readers", "dsem", "dcnt")

    def __init__(self, name):
        self.name = name
        self.writer = None
        self.readers = {}
        self.dsem = None
        self.dcnt = 0


class Sched:
    def __init__(self, nc, es):
        self.nc = nc
        self.es = es
        self.E = {"pe": nc.tensor, "act": nc.scalar, "dve": nc.vector, "pool": nc.gpsimd, "sp": nc.sync}
        self.sem = {k: es.enter_context(nc.semaphore("s_" + k)) for k in self.E}
        self.cnt = {k: 0 for k in self.E}
        self.seen = {k: {} for k in self.E}
        self.nbuf = 0
        self.dtoks = {}
        self.dsems = {}

    def buf(self, name=None):
        self.nbuf += 1
        return Buf(name or f"b{self.nbuf}")

    def _wait(self, eng, tok):
        sem, val = tok
        if eng == "pe" and sem is self.sem["pe"]:
            return
        key = id(sem)
        if self.seen[eng].get(key, 0) < val:
            self.E[eng].wait_ge(sem, val)
            self.seen[eng][key] = val

    def _deps(self, eng, reads, writes):
        for b in reads:
            if b.writer is not None:
                self._wait(eng, b.writer)
        for b in writes:
            if b.writer is not None:
                self._wait(eng, b.writer)
            for tok in list(b.readers.values()):
                self._wait(eng, tok)

    def _commit(self, tok, reads, writes):
        k = id(tok[0])
        for b in reads:
            if k not in b.readers or b.readers[k][1] < tok[1]:
                b.readers[k] = tok
        for b in writes:
            b.writer = tok
            b.readers = {}

    def op(self, eng, fn, reads=(), writes=()):
        self._deps(eng, reads, writes)
        ins = fn(self.E[eng])
        self.cnt[eng] += 1
        ins.then_inc(self.sem[eng], 1)
        tok = (self.sem[eng], self.cnt[eng])
        self._commit(tok, reads, writes)
        return tok

    def dma(self, eng, out, in_, reads=(), writes=()):
        self._deps(eng, reads, writes)
        b = writes[0]
        if b.name not in self.dsems:
            self.dsems[b.name] = [self.es.enter_context(self.nc.semaphore("d_" + b.name)), 0]
        ent = self.dsems[b.name]
        ent[1] += 16
        b.dsem = ent[0]
        b.dcnt = ent[1]
        self.E[eng].dma_start(out=out, in_=in_).then_inc(b.dsem, 16)
        tok = (b.dsem, b.dcnt)
        self.dtoks[id(b.dsem)] = tok
        self._commit(tok, reads, writes)
        return tok

    def barrier(self):
        toks = [(self.sem[k], self.cnt[k]) for k in self.E if self.cnt[k] > 0]
        toks += list(self.dtoks.values())
        for eng in self.E:
            for tok in toks:
                if tok[0] is self.sem[eng]:
                    continue
                self._wait(eng, tok)

    def finish(self, eng, bufs):
        for b in bufs:
            if b.writer is not None:
                self._wait(eng, b.writer)


class T:
    def __init__(self, sc, es, name, shape, dtype, psum=False):
        nc = sc.nc
        base = name
        sc.nbuf += 1
        name = f"{name}_{sc.nbuf}"
        if psum:
            self.t = es.enter_context(nc.psum_tensor(name, shape, dtype))
        else:
            self.t = es.enter_context(nc.sbuf_tensor(name, shape, dtype))
        self.b = sc.buf(base)

    def __getitem__(self, k):
        return self.t[k]


def mixer_layer(nc, sc, TT, nextps, PS, l, xT, xb, hT, ident, modT, gsc1, norm_modulate, dd, dbg):
    norm_modulate(l, gsc1, 0)
    w_in_d = dd["w_in"]

    def wload(dst, c0, n):
        sc.dma("pool", dst[:], w_in_d[l, :, c0:c0 + n].rearrange("(k p) c -> p k c", p=128), writes=[dst.b])

    with ExitStack() as esm:
        ogT = TT("ogT", [128, 4, S], BF16, scope=esm)

        with ExitStack() as eh:
          if dbg in (None, "hgrn"):
                lbc = TT("lbc", [128, 8], F32, scope=eh)
                omlc = TT("omlc", [128, 8], F32, scope=eh)
                omlr = TT("omlr", [128, 1024], F32, scope=eh)
                with ExitStack() as e2:
                    lc = TT("lc", [128, 4, 8], F32, scope=e2)
                    lr = TT("lr", [128, 4, 1024], F32, scope=e2)
                    dc = TT("dc", [128, 8], F32, scope=e2)
                    dr = TT("dr", [128, 1024], F32, scope=e2)
                    sc.dma("sp", lc[:], dd["lbl_col"], writes=[lc.b])
                    sc.dma("sp", lr[:], dd["lbl_row"], writes=[lr.b])
                    for (src, den, num) in ((lc, dc, lbc), (lr, dr, omlr)):
                        sc.op("act", lambda e, src=src: e.activation(out=src[:], in_=src[:], func=AF.Exp), reads=[src.b], writes=[src.b])
                        sc.op("dve", lambda e, src=src, den=den: e.tensor_tensor(out=den[:], in0=src[:, 0, :], in1=src[:, 1, :], op=ALU.add),
                              reads=[src.b], writes=[den.b])
                        for j in (2, 3):
                            sc.op("dve", lambda e, src=src, den=den, j=j: e.tensor_tensor(out=den[:], in0=den[:], in1=src[:, j, :], op=ALU.add),
                                  reads=[src.b, den.b], writes=[den.b])
                        sc.op("dve", lambda e, den=den: e.reciprocal(out=den[:], in_=den[:]), reads=[den.b], writes=[den.b])
                        if l == 0:
                            sc.op("dve", lambda e, num=num: e.memset(num[:], 0.0), writes=[num.b])
                        else:
                            sc.op("dve", lambda e, src=src, num=num: e.tensor_copy(out=num[:], in_=src[:, 1, :]), reads=[src.b], writes=[num.b])
                            for j in range(2, l + 1):
                                sc.op("dve", lambda e, src=src, num=num, j=j: e.tensor_tensor(out=num[:], in0=num[:], in1=src[:, j, :], op=ALU.add),
                                      reads=[src.b, num.b], writes=[num.b])
                            sc.op("dve", lambda e, num=num, den=den: e.tensor_tensor(out=num[:], in0=num[:], in1=den[:], op=ALU.mult),
                                  reads=[num.b, den.b], writes=[num.b])
                    sc.op("dve", lambda e: e.tensor_scalar(out=omlc[:], in0=lbc[:], scalar1=-1.0, scalar2=1.0, op0=ALU.mult, op1=ALU.add),
                          reads=[lbc.b], writes=[omlc.b])
                    sc.op("dve", lambda e: e.tensor_scalar(out=omlr[:], in0=omlr[:], scalar1=-1.0, scalar2=1.0, op0=ALU.mult, op1=ALU.add),
                          reads=[omlr.b], writes=[omlr.b])
                    sc.barrier()

                hg = TT("hg", [128, 128], F32, scope=eh)
                uext = TT("uext_sb", [128, 2, 67], F32, scope=eh)
                msk = TT("mask_sb", [128, 2, 64], F32, scope=eh)
                sc.dma("sp", hg[:], dd["hgain"][:, l, :], writes=[hg.b])
                sc.dma("sp", uext[:], dd["uext"], writes=[uext.b])
                sc.dma("sp", msk[:], dd["mask"], writes=[msk.b])
                wq = TT("wq", [128, KD, 128], BF16, scope=eh)
                wf = [TT(f"wf{i}", [128, KD, 128], BF16, scope=eh) for i in range(2)]
                wi = TT("wi", [128, KD, 128], BF16, scope=eh)
                wgt = TT("wgt", [128, KD, 128], BF16, scope=eh)
                qT = TT("qT", [128, S], F32, scope=eh)
                kT = TT("kT", [128, S], F32, scope=eh)
                gTM = TT("gTM", [128, NT, 128], F32, scope=eh)
                kTM = TT("kTM", [128, NT, 128], F32, scope=eh)
                vTM = TT("vTM", [128, NT, 128], BF16, scope=eh)
                sgTM = TT("sgTM", [128, NT, 128], F32, scope=eh)
                oacc = TT("oacc", [128, NT, 128], F32, scope=eh)
                e1 = [TT(f"e1_{i}", [128, 64], F32, scope=eh) for i in range(2)]
                e2t = [TT(f"e2t_{i}", [128, 64], F32, scope=eh) for i in range(2)]
                e2 = [TT(f"e2_{i}", [128, 128], F32, scope=eh) for i in range(2)]
                em3 = [TT(f"em3_{i}", [128, 3], F32, scope=eh) for i in range(2)]
                QbT = [TT(f"QbT{i}", [128, 64], BF16, scope=eh) for i in range(2)]
                KbT = [TT(f"KbT{i}", [128, 64], BF16, scope=eh) for i in range(2)]
                Kb = [TT(f"Kb{i}", [128, 128], BF16, scope=eh) for i in range(2)]
                ATm = [TT(f"ATm{i}", [128, 64], BF16, scope=eh) for i in range(2)]
                Sst = TT("Sst", [128, 128], F32, scope=eh)
                Sp = [TT(f"Sp{i}", [128, 128], BF16, scope=eh) for i in range(2)]
                ssq = TT("hssq", [128, NT], F32, scope=eh)

                for h in range(4):
                    wload(wq, h * 128, 128)
                    wload(wf[0], 512 + h * 128, 128)
                    wload(wf[1], 1024 + h * 128, 128)
                    wload(wi, 1536 + h * 128, 128)
                    wload(wgt, 2048 + h * 128, 128)
                    for blk in range(NB):
                        ts = slice(blk * 512, (blk + 1) * 512)
                        ps = nextps()

                        def mmq(e, ps=ps, ts=ts):
                            ins = None
                            for k in range(KD):
                                ins = e.matmul(ps[:, :], lhsT=wq[:, k, :], rhs=hT[:, k, ts], start=(k == 0), stop=(k == KD - 1))
                            return ins
                        sc.op("pe", mmq, reads=[wq.b, hT.b], writes=[ps.b])
                        sc.op("act", lambda e, ps=ps, ts=ts: e.activation(out=qT[:, ts], in_=ps[:, :], func=AF.Silu), reads=[ps.b], writes=[qT.b])
                    for t in range(NT):
                        tsl = slice(t * 128, (t + 1) * 128)
                        ps = nextps()

                        def mmv(e, ps=ps, tsl=tsl):
                            ins = None
                            for gi_, wt in enumerate((wi, wgt)):
                                for k in range(KD):
                                    ins = e.matmul(ps[:, gi_ * 128:(gi_ + 1) * 128], lhsT=hT[:, k, tsl], rhs=wt[:, k, :],
                                                   start=(k == 0), stop=(k == KD - 1))
                            return ins
                        sc.op("pe", mmv, reads=[wi.b, wgt.b, hT.b], writes=[ps.b])
                        sc.op("act", lambda e, ps=ps, t=t: e.copy(out=vTM[:, t, :], in_=ps[:, 0:128]), reads=[ps.b], writes=[vTM.b])
                        sc.op("act", lambda e, ps=ps, t=t: e.activation(out=sgTM[:, t, :], in_=ps[:, 128:256], func=AF.Sigmoid),
                              reads=[ps.b], writes=[sgTM.b])
                    for dr_ in range(2):
                        w_f = wf[dr_]
                        col = dr_ * 4 + h
                        rsl = slice(dr_ * 512 + h * 128, dr_ * 512 + (h + 1) * 128)
                        for blk in range(NB):
                            ts = slice(blk * 512, (blk + 1) * 512)
                            ps = nextps()

                            def mmk(e, ps=ps, ts=ts, w_f=w_f):
                                ins = None
                                for k in range(KD):
                                    ins = e.matmul(ps[:, :], lhsT=w_f[:, k, :], rhs=hT[:, k, ts], start=(k == 0), stop=(k == KD - 1))
                                return ins
                            sc.op("pe", mmk, reads=[w_f.b, hT.b], writes=[ps.b])
                            sc.op("act", lambda e, ps=ps, ts=ts: e.activation(out=kT[:, ts], in_=ps[:, :], func=AF.Sigmoid, scale=-1.0),
                                  reads=[ps.b], writes=[kT.b])
                            sc.op("dve", lambda e, ts=ts, col=col: e.tensor_scalar(out=kT[:, ts], in0=kT[:, ts], scalar1=omlc[:, col:col + 1],
                                                                                  scalar2=None, op0=ALU.mult), reads=[kT.b, omlc.b], writes=[kT.b])
                        for t in range(NT):
                            tsl = slice(t * 128, (t + 1) * 128)
                            ps = nextps()

                            def mmf(e, ps=ps, tsl=tsl, w_f=w_f):
                                ins = None
                                for k in range(KD):
                                    ins = e.matmul(ps[:, 0:128], lhsT=hT[:, k, tsl], rhs=w_f[:, k, :], start=(k == 0), stop=(k == KD - 1))
                                return ins
                            sc.op("pe", mmf, reads=[w_f.b, hT.b], writes=[ps.b])
                            sc.op("act", lambda e, ps=ps, t=t: e.activation(out=kTM[:, t, :], in_=ps[:, 0:128], func=AF.Sigmoid, scale=-1.0),
                                  reads=[ps.b], writes=[kTM.b])
                            sc.op("dve", lambda e, t=t, rsl=rsl: e.tensor_tensor(out=kTM[:, t, :], in0=kTM[:, t, :], in1=omlr[:, rsl], op=ALU.mult),
                                  reads=[kTM.b, omlr.b], writes=[kTM.b])
                            sc.op("act", lambda e, t=t: e.activation(out=gTM[:, t, :], in_=kTM[:, t, :], func=AF.Ln, scale=-1.0, bias=1.0),
                                  reads=[kTM.b], writes=[gTM.b])
                        order = list(range(32)) if dr_ == 0 else list(range(31, -1, -1))
                        for a_ in ATm:
                            sc.op("dve", lambda e, a_=a_: e.memset(a_[:], 0.0), reads=[a_.b], writes=[a_.b])
                        for ci, c in enumerate(order):
                            t, half = divmod(c, 2)
                            hr = slice(half * 64, half * 64 + 64)
                            cc = slice(c * 64, (c + 1) * 64)
                            bA = PS[2 * (ci % 4)]
                            bB = PS[2 * (ci % 4) + 1]
                            i2 = ci % 2
                            first = (ci == 0)

                            def mmcs(e, bA=bA, hr=hr, t=t):
                                e.matmul(bA[:, 0:67], lhsT=gTM[hr, t, :], rhs=uext[hr, dr_, :], start=True, stop=True)
                                return e.matmul(bA[hr, 128:256], lhsT=uext[hr, dr_, 0:64], rhs=gTM[hr, t, :], start=True, stop=True)
                            sc.op("pe", mmcs, reads=[gTM.b, uext.b], writes=[bA.b])
                            sc.op("act", lambda e, bA=bA, i2=i2: e.activation(out=e1[i2][:], in_=bA[:, 0:64], func=AF.Exp), reads=[bA.b], writes=[e1[i2].b])
                            sc.op("act", lambda e, bA=bA, i2=i2: e.activation(out=e2t[i2][:], in_=bA[:, 0:64], func=AF.Exp, scale=-1.0),
                                  reads=[bA.b], writes=[e2t[i2].b])
                            sc.op("act", lambda e, bA=bA, i2=i2, hr=hr: e.activation(out=e2[i2][hr, :], in_=bA[hr, 128:256], func=AF.Exp, scale=-1.0),
                                  reads=[bA.b], writes=[e2[i2].b])
                            sc.op("act", lambda e, bA=bA, i2=i2: e.activation(out=em3[i2][:], in_=bA[:, 64:67], func=AF.Exp), reads=[bA.b], writes=[em3[i2].b])
                            sc.op("dve", lambda e, i2=i2, cc=cc: e.tensor_tensor(out=QbT[i2][:], in0=qT[:, cc], in1=e1[i2][:], op=ALU.mult),
                                  reads=[qT.b, e1[i2].b], writes=[QbT[i2].b])
                            sc.op("dve", lambda e, i2=i2, cc=cc: e.tensor_tensor(out=KbT[i2][:], in0=kT[:, cc], in1=e2t[i2][:], op=ALU.mult),
                                  reads=[kT.b, e2t[i2].b], writes=[KbT[i2].b])
                            sc.op("dve", lambda e, i2=i2, hr=hr, t=t: e.tensor_tensor(out=Kb[i2][hr, :], in0=kTM[hr, t, :], in1=e2[i2][hr, :], op=ALU.mult),
                                  reads=[kTM.b, e2[i2].b], writes=[Kb[i2].b])
                            sc.op("pe", lambda e, bA=bA, hr=hr, i2=i2: e.matmul(bA[hr, 256:320], lhsT=KbT[i2][:], rhs=QbT[i2][:], start=True, stop=True),
                                  reads=[KbT[i2].b, QbT[i2].b], writes=[bA.b])
                            sc.op("dve", lambda e, bA=bA, hr=hr, i2=i2: e.copy_predicated(out=ATm[i2][hr, :], mask=msk[hr, dr_, :].bitcast(mybir.dt.uint32), data=bA[hr, 256:320]),
                                  reads=[bA.b, msk.b, ATm[i2].b], writes=[ATm[i2].b])
                            if not first:
                                sc.op("dve", lambda e, i2=i2: e.tensor_scalar(out=Sp[i2][:], in0=Sst[:], scalar1=em3[i2][:, 0:1], scalar2=None, op0=ALU.mult),
                                      reads=[Sst.b, em3[i2].b], writes=[Sp[i2].b])

                            def mmo(e, bB=bB, bA=bA, hr=hr, t=t, i2=i2, first=first):
                                ins = e.matmul(bB[hr, 0:128], lhsT=ATm[i2][hr, :], rhs=vTM[hr, t, :], start=True, stop=first)
                                if not first:
                                    ins = e.matmul(bB[hr, 0:128], lhsT=QbT[i2][:], rhs=Sp[i2][:], start=False, stop=True)
                                ins2 = e.matmul(bA[:, 384:512], lhsT=Kb[i2][hr, :], rhs=vTM[hr, t, :], start=True, stop=True)
                                return ins2
                            sc.op("pe", mmo, reads=[ATm[i2].b, vTM.b, QbT[i2].b, Sp[i2].b, Kb[i2].b], writes=[bB.b, bA.b])
                            if dr_ == 0:
                                sc.op("act", lambda e, bB=bB, hr=hr, t=t: e.copy(out=oacc[hr, t, :], in_=bB[hr, 0:128]), reads=[bB.b], writes=[oacc.b])
                            else:
                                sc.op("dve", lambda e, bB=bB, hr=hr, t=t: e.tensor_tensor(out=oacc[hr, t, :], in0=oacc[hr, t, :], in1=bB[hr, 0:128], op=ALU.add),
                                      reads=[bB.b, oacc.b], writes=[oacc.b])
                            if first:
                                sc.op("dve", lambda e, bA=bA, i2=i2: e.tensor_scalar(out=Sst[:], in0=bA[:, 384:512], scalar1=em3[i2][:, 2:3], scalar2=None, op0=ALU.mult),
                                      reads=[bA.b, em3[i2].b], writes=[Sst.b])
                            else:
                                sc.op("dve", lambda e, i2=i2: e.tensor_scalar(out=Sst[:], in0=Sst[:], scalar1=em3[i2][:, 1:2], scalar2=None, op0=ALU.mult),
                                      reads=[Sst.b, em3[i2].b], writes=[Sst.b])
                                sc.op("dve", lambda e, bA=bA, i2=i2: e.scalar_tensor_tensor(out=Sst[:], in0=bA[:, 384:512], scalar=em3[i2][:, 2:3], in1=Sst[:],
                                                                                            op0=ALU.mult, op1=ALU.add),
                                      reads=[bA.b, em3[i2].b, Sst.b], writes=[Sst.b])
                    sq3 = kTM
                    sc.op("dve", lambda e: e.tensor_tensor(out=sq3[:], in0=oacc[:], in1=oacc[:], op=ALU.mult), reads=[oacc.b], writes=[sq3.b])
                    sc.op("dve", lambda e: e.reduce_sum(out=ssq[:], in_=sq3[:], axis=AX.X), reads=[sq3.b], writes=[ssq.b])
                    sc.op("dve", lambda e: e.tensor_scalar(out=ssq[:], in0=ssq[:], scalar1=1.0 / 128, scalar2=EPS, op0=ALU.mult, op1=ALU.add),
                          reads=[ssq.b], writes=[ssq.b])
                    sc.op("act", lambda e: e.activation(out=ssq[:], in_=ssq[:], func=AF.Sqrt), reads=[ssq.b], writes=[ssq.b])
                    sc.op("dve", lambda e: e.reciprocal(out=ssq[:], in_=ssq[:]), reads=[ssq.b], writes=[ssq.b])
                    sc.op("dve", lambda e: e.tensor_tensor(out=oacc[:], in0=oacc[:], in1=ssq[:].unsqueeze(2).to_broadcast([128, NT, 128]), op=ALU.mult),
                          reads=[oacc.b, ssq.b], writes=[oacc.b])
                    sc.op("dve", lambda e: e.tensor_tensor(out=oacc[:], in0=oacc[:], in1=hg[:].unsqueeze(1).to_broadcast([128, NT, 128]), op=ALU.mult),
                          reads=[oacc.b, hg.b], writes=[oacc.b])
                    sc.op("dve", lambda e: e.tensor_tensor(out=oacc[:], in0=oacc[:], in1=sgTM[:], op=ALU.mult),
                          reads=[oacc.b, sgTM.b], writes=[oacc.b])
                    for tg in range(4):
                        ps = nextps()

                        def trh(e, ps=ps, tg=tg):
                            ins = None
                            for j in range(4):
                                ins = e.transpose(ps[:, j * 128:(j + 1) * 128], oacc[:, tg * 4 + j, :], ident[:])
                            return ins
                        sc.op("pe", trh, reads=[oacc.b, ident.b], writes=[ps.b])
                        sc.op("act", lambda e, ps=ps, tg=tg, h=h: e.copy(out=ogT[:, h, tg * 512:(tg + 1) * 512], in_=ps[:, :]), reads=[ps.b], writes=[ogT.b])
                sc.barrier()

        if dbg == "hgrn":
            sc.op("dve", lambda e: e.tensor_copy(out=xT[:, 0:4, :], in_=ogT[:]), reads=[ogT.b] + xb[0:4], writes=xb[0:4])
            sc.barrier()
            return
        mlaT = TT("mlaT", [128, 4, S], BF16, scope=esm)
        with ExitStack() as em:
            cosT = TT("cosT", [128, S], BF16, scope=em)
            sinT = TT("sinT", [128, S], BF16, scope=em)
            cqT = TT("cqT", [128, 3, S], BF16, scope=em)
            ckvT = TT("ckvT", [128, 2, S], BF16, scope=em)
            kpeT = TT("kpeT", [128, S], BF16, scope=em)
            with ExitStack() as e2:
                posi = TT("posi", [128, S], I32, scope=e2)
                u = TT("ropeu", [128, S], F32, scope=e2)
                nf = TT("ropen", [128, S], F32, scope=e2)
                ni = TT("ropeni", [128, S], I32, scope=e2)
                fq = TT("fq", [128, 1], F32, scope=e2)
                sc.dma("sp", posi[:], dd["pos"], writes=[posi.b])
                sc.dma("sp", fq[:], dd["freq"], writes=[fq.b])
                sc.op("dve", lambda e: e.tensor_copy(out=u[:], in_=posi[:]), reads=[posi.b], writes=[u.b])
                sc.op("dve", lambda e: e.tensor_scalar(out=u[:], in0=u[:], scalar1=fq[:, 0:1], scalar2=None, op0=ALU.mult), reads=[u.b, fq.b], writes=[u.b])
                sc.op("dve", lambda e: e.tensor_copy(out=ni[:], in_=u[:]), reads=[u.b], writes=[ni.b])
                sc.op("dve", lambda e: e.tensor_copy(out=nf[:], in_=ni[:]), reads=[ni.b], writes=[nf.b])
                sc.op("dve", lambda e: e.tensor_tensor(out=u[:], in0=u[:], in1=nf[:], op=ALU.subtract), reads=[u.b, nf.b], writes=[u.b])

                def wrap(tt):
                    sc.op("dve", lambda e: e.tensor_scalar(out=nf[:], in0=tt[:], scalar1=0.5, scalar2=None, op0=ALU.is_ge), reads=[tt.b], writes=[nf.b])
                    sc.op("dve", lambda e: e.tensor_tensor(out=tt[:], in0=tt[:], in1=nf[:], op=ALU.subtract), reads=[tt.b, nf.b], writes=[tt.b])
                    sc.op("dve", lambda e: e.tensor_scalar(out=nf[:], in0=tt[:], scalar1=-0.5, scalar2=None, op0=ALU.is_lt), reads=[tt.b], writes=[nf.b])
                    sc.op("dve", lambda e: e.tensor_tensor(out=tt[:], in0=tt[:], in1=nf[:], op=ALU.add), reads=[tt.b, nf.b], writes=[tt.b])
                wrap(u)
                sc.op("act", lambda e: e.activation(out=sinT[:], in_=u[:], func=AF.Sin, scale=6.28318), reads=[u.b], writes=[sinT.b])
                sc.op("dve", lambda e: e.tensor_scalar(out=u[:], in0=u[:], scalar1=0.25, scalar2=None, op0=ALU.add), reads=[u.b, sinT.b], writes=[u.b])
                wrap(u)
                sc.op("act", lambda e: e.activation(out=cosT[:], in_=u[:], func=AF.Sin, scale=6.28318), reads=[u.b], writes=[cosT.b])
                sc.barrier()
            with ExitStack() as e2:
                wcq = TT("wcq", [128, KD, 384], BF16, scope=e2)
                wckv = TT("wckv", [128, KD, 256], BF16, scope=e2)
                wkpe = TT("wkpe", [128, KD, 32], BF16, scope=e2)
                wkps = TT("wkps", [128, KD, 32], BF16, scope=e2)
                qng = TT("qng", [128, 3], F32, scope=e2)
                kvng = TT("kvng", [128, 2], F32, scope=e2)
                wload(wcq, 2560, 384)
                wload(wckv, 2944, 256)
                wload(wkpe, 3200, 32)
                sc.dma("sp", qng[:], dd["qng"][:, l, :], writes=[qng.b])
                sc.dma("sp", kvng[:], dd["kvng"][:, l, :], writes=[kvng.b])
                sc.op("dve", lambda e: e.tensor_scalar(out=wkps[:, :, 0:16], in0=wkpe[:, :, 16:32], scalar1=-1.0, scalar2=None, op0=ALU.mult),
                      reads=[wkpe.b], writes=[wkps.b])
                sc.op("dve", lambda e: e.tensor_copy(out=wkps[:, :, 16:32], in_=wkpe[:, :, 0:16]), reads=[wkpe.b, wkps.b], writes=[wkps.b])
                junk = TT("junk", [128, 384], F32, scope=e2)
                cs_ = [TT(f"cqs{i}", [128, 384], F32, scope=e2) for i in range(2)]
                rs = [TT(f"crs{i}", [128, 1], F32, scope=e2) for i in range(2)]
                ci_ = 0
                for t in range(NT):
                    tsl = slice(t * 128, (t + 1) * 128)
                    for (wt, n, gn, dst, nk) in ((wcq, 384, qng, cqT, 3), (wckv, 256, kvng, ckvT, 2)):
                        ps = nextps()

                        def mmc(e, ps=ps, wt=wt, n=n, tsl=tsl):
                            ins = None
                            for k in range(KD):
                                ins = e.matmul(ps[:, 0:n], lhsT=hT[:, k, tsl], rhs=wt[:, k, :], start=(k == 0), stop=(k == KD - 1))
                            return ins
                        sc.op("pe", mmc, reads=[wt.b, hT.b], writes=[ps.b])
                        r = rs[ci_ % 2]
                        cq = cs_[ci_ % 2]
                        ci_ += 1
                        sc.op("act", lambda e, ps=ps, n=n, r=r: e.activation(out=junk[:, 0:n], in_=ps[:, 0:n], func=AF.Square, accum_out=r[:, 0:1]),
                              reads=[ps.b], writes=[junk.b, r.b])
                        sc.op("dve", lambda e, r=r, n=n: e.tensor_scalar(out=r[:], in0=r[:], scalar1=1.0 / n, scalar2=EPS, op0=ALU.mult, op1=ALU.add),
                              reads=[r.b], writes=[r.b])
                        sc.op("act", lambda e, r=r: e.activation(out=r[:], in_=r[:], func=AF.Sqrt), reads=[r.b], writes=[r.b])
                        sc.op("dve", lambda e, r=r: e.reciprocal(out=r[:], in_=r[:]), reads=[r.b], writes=[r.b])
                        sc.op("dve", lambda e, ps=ps, n=n, r=r, cq=cq: e.tensor_scalar(out=cq[:, 0:n], in0=ps[:, 0:n], scalar1=r[:, 0:1], scalar2=None, op0=ALU.mult),
                              reads=[ps.b, r.b], writes=[cq.b])
                        pt = nextps()

                        def trc(e, pt=pt, cq=cq, nk=nk):
                            ins = None
                            for j in range(nk):
                                ins = e.transpose(pt[:, j * 128:(j + 1) * 128], cq[:, j * 128:(j + 1) * 128], ident[:])
                            return ins
                        sc.op("pe", trc, reads=[cq.b, ident.b], writes=[pt.b])
                        sc.op("dve", lambda e, pt=pt, nk=nk, gn=gn, dst=dst, tsl=tsl: e.tensor_tensor(
                            out=dst[:, :, tsl], in0=pt[:, 0:nk * 128].rearrange("p (j c) -> p j c", c=128),
                            in1=gn[:].unsqueeze(2).to_broadcast([128, nk, 128]), op=ALU.mult), reads=[pt.b, gn.b], writes=[dst.b])
                t1 = TT("kt1", [128, 512], F32, scope=e2)
                t2 = TT("kt2", [128, 512], F32, scope=e2)
                R = slice(64, 96)
                for blk in range(NB):
                    ts = slice(blk * 512, (blk + 1) * 512)
                    ps = nextps()
                    ps2 = nextps()

                    def mmp(e, pp, wt, ts=ts):
                        ins = None
                        for k in range(KD):
                            ins = e.matmul(pp[R, :], lhsT=wt[:, k, :], rhs=hT[:, k, ts], start=(k == 0), stop=(k == KD - 1))
                        return ins
                    sc.op("pe", lambda e, ps=ps: mmp(e, ps, wkpe), reads=[wkpe.b, hT.b], writes=[ps.b])
                    sc.op("pe", lambda e, ps2=ps2: mmp(e, ps2, wkps), reads=[wkps.b, hT.b], writes=[ps2.b])
                    sc.op("dve", lambda e, ps=ps, ts=ts: e.tensor_tensor(out=t1[R, :], in0=ps[R, :], in1=cosT[R, ts], op=ALU.mult), reads=[ps.b, cosT.b], writes=[t1.b])
                    sc.op("dve", lambda e, ps2=ps2, ts=ts: e.tensor_tensor(out=t2[R, :], in0=ps2[R, :], in1=sinT[R, ts], op=ALU.mult), reads=[ps2.b, sinT.b], writes=[t2.b])
                    sc.op("dve", lambda e, ts=ts: e.tensor_tensor(out=kpeT[R, ts], in0=t1[R, :], in1=t2[R, :], op=ALU.add), reads=[t1.b, t2.b], writes=[kpeT.b])
                sc.barrier()
            with ExitStack() as e2:
                wqu = TT("wqu", [128, 3, 768], BF16, scope=e2)
                wqs = TT("wqs", [128, 3, 768], BF16, scope=e2)
                wkv = TT("wkv", [128, 2, 1024], BF16, scope=e2)
                sc.dma("pool", wqu[:], dd["w_qup"][l].rearrange("(k p) c -> p k c", p=128), writes=[wqu.b])
                sc.dma("pool", wkv[:], dd["w_kvup"][l].rearrange("(k p) c -> p k c", p=128), writes=[wkv.b])
                sc.op("dve", lambda e: e.tensor_copy(out=wqs[:], in_=wqu[:]), reads=[wqu.b], writes=[wqs.b])
                v4 = lambda tt: tt[:].rearrange("p k (h c) -> p k h c", c=96)
                sc.op("dve", lambda e: e.tensor_scalar(out=v4(wqs)[:, :, :, 64:80], in0=v4(wqu)[:, :, :, 80:96], scalar1=-1.0, scalar2=None, op0=ALU.mult),
                      reads=[wqu.b, wqs.b], writes=[wqs.b])
                sc.op("dve", lambda e: e.tensor_copy(out=v4(wqs)[:, :, :, 80:96], in_=v4(wqu)[:, :, :, 64:80]), reads=[wqu.b, wqs.b], writes=[wqs.b])
                vh = [TT(f"vh{i}", [128, NT, 64], BF16, scope=e2) for i in range(2)]
                ones64 = TT("ones64", [128, 64], BF16, scope=e2)
                sc.op("dve", lambda e: e.memset(ones64[:], 1.0), writes=[ones64.b])
                kfull = [TT(f"kfull{i}", [128, S], BF16, scope=e2) for i in range(2)]
                qrot = [TT(f"qrot{i}", [128, 512], BF16, scope=e2) for i in range(2)]
                pT = [TT(f"pT{i}", [128, 512], BF16, scope=e2) for i in range(4)]
                t1 = TT("qt1", [128, 512], F32, scope=e2)
                t2 = TT("qt2", [128, 512], F32, scope=e2)
                rden = TT("rden", [128, 512], F32, scope=e2)
                scale = 96.0 ** -0.5
                Q = slice(0, 96)
                pti = 0
                qi = 0
                for h in range(8):
                    kf = kfull[h % 2]
                    vv = vh[h % 2]
                    hp = slice((h % 2) * 64, (h % 2) * 64 + 64)
                    for blk in range(NB):
                        ts = slice(blk * 512, (blk + 1) * 512)
                        ps = nextps()

                        def mmkn(e, ps=ps, ts=ts, h=h):
                            ins = None
                            for k in range(2):
                                ins = e.matmul(ps[0:64, :], lhsT=wkv[:, k, h * 128:h * 128 + 64], rhs=ckvT[:, k, ts], start=(k == 0), stop=(k == 1))
                            return ins
                        sc.op("pe", mmkn, reads=[wkv.b, ckvT.b], writes=[ps.b])
                        sc.op("dve", lambda e, ps=ps, ts=ts, kf=kf: e.tensor_copy(out=kf[0:64, ts], in_=ps[0:64, :]), reads=[ps.b], writes=[kf.b])
                    sc.op("pool", lambda e, kf=kf: e.tensor_copy(out=kf[64:96, :], in_=kpeT[64:96, :]), reads=[kpeT.b, kf.b], writes=[kf.b])
                    for t in range(NT):
                        tsl = slice(t * 128, (t + 1) * 128)
                        ps = nextps()

                        def mmvv(e, ps=ps, tsl=tsl, h=h):
                            ins = None
                            for k in range(2):
                                ins = e.matmul(ps[:, 0:64], lhsT=ckvT[:, k, tsl], rhs=wkv[:, k, h * 128 + 64:h * 128 + 128], start=(k == 0), stop=(k == 1))
                            return ins
                        sc.op("pe", mmvv, reads=[wkv.b, ckvT.b], writes=[ps.b])
                        sc.op("dve", lambda e, ps=ps, t=t, vv=vv: e.tensor_copy(out=vv[:, t, :], in_=ps[:, 0:64]), reads=[ps.b], writes=[vv.b])
                    for blk in range(NB):
                        ts = slice(blk * 512, (blk + 1) * 512)
                        ps = PS[0]
                        ps2 = PS[1]

                        def mmqq(e, pp, wt, ts=ts, h=h):
                            ins = None
                            for k in range(3):
                                ins = e.matmul(pp[Q, :], lhsT=wt[:, k, h * 96:(h + 1) * 96], rhs=cqT[:, k, ts], start=(k == 0), stop=(k == 2))
                            return ins
                        sc.op("pe", lambda e, ps=ps: mmqq(e, ps, wqu), reads=[wqu.b, cqT.b], writes=[ps.b])
                        sc.op("pe", lambda e, ps2=ps2: mmqq(e, ps2, wqs), reads=[wqs.b, cqT.b], writes=[ps2.b])
                        qr = qrot[qi % 2]
                        qi += 1
                        sc.op("dve", lambda e, ps=ps, ts=ts: e.tensor_tensor(out=t1[Q, :], in0=ps[Q, :], in1=cosT[Q, ts], op=ALU.mult), reads=[ps.b, cosT.b], writes=[t1.b])
                        sc.op("dve", lambda e, ps2=ps2, ts=ts: e.tensor_tensor(out=t2[Q, :], in0=ps2[Q, :], in1=sinT[Q, ts], op=ALU.mult), reads=[ps2.b, sinT.b], writes=[t2.b])
                        sc.op("dve", lambda e, qr=qr: e.tensor_tensor(out=qr[Q, :], in0=t1[Q, :], in1=t2[Q, :], op=ALU.add), reads=[t1.b, t2.b], writes=[qr.b])
                        pn = PS[4 + (qi % 2)]
                        pd = PS[6 + (qi % 2)]
                        LA2 = 2
                        pbuf = {}
                        for step in range(NT + LA2):
                            if step < NT:
                                kt = step
                                ksl = slice(kt * 128, (kt + 1) * 128)
                                sps = PS[kt % 4]
                                p_ = pT[pti % 4]
                                pti += 1
                                pbuf[kt] = p_
                                sc.op("pe", lambda e, sps=sps, ksl=ksl, kf=kf, qr=qr: e.matmul(sps[:, :], lhsT=kf[Q, ksl], rhs=qr[Q, :], start=True, stop=True),
                                      reads=[kf.b, qr.b], writes=[sps.b])
                                sc.op("act", lambda e, sps=sps, p_=p_: e.activation(out=p_[:], in_=sps[:, :], func=AF.Exp, scale=scale), reads=[sps.b], writes=[p_.b])
                            if step >= LA2:
                                kt = step - LA2
                                p_ = pbuf[kt]

                                def mmpv(e, p_=p_, kt=kt, vv=vv, pn=pn, pd=pd, hp=hp):
                                    e.matmul(pn[hp, :], lhsT=vv[:, kt, :], rhs=p_[:], start=(kt == 0), stop=(kt == NT - 1))
                                    return e.matmul(pd[hp, :], lhsT=ones64[:], rhs=p_[:], start=(kt == 0), stop=(kt == NT - 1))
                                sc.op("pe", mmpv, reads=[p_.b, vv.b, ones64.b], writes=[pn.b, pd.b])
                        sc.op("dve", lambda e, pd=pd, hp=hp: e.reciprocal(out=rden[hp, :], in_=pd[hp, :]), reads=[pd.b], writes=[rden.b])
                        sc.op("dve", lambda e, pn=pn, hp=hp, h=h, ts=ts: e.tensor_tensor(out=mlaT[hp, h // 2, ts], in0=pn[hp, :], in1=rden[hp, :], op=ALU.mult),
                              reads=[pn.b, rden.b], writes=[mlaT.b])
                sc.barrier()
            sc.barrier()

        if dbg == "mla":
            sc.op("dve", lambda e: e.tensor_copy(out=xT[:, 0:4, :], in_=mlaT[:]), reads=[mlaT.b] + xb[0:4], writes=xb[0:4])
            sc.barrier()
            return
        with ExitStack() as ec:
            yT = TT("yT", [128, KD, S], BF16, scope=ec)
            woh = TT("woh", [128, 4, D], BF16, scope=ec)
            wom = TT("wom", [128, 4, D], BF16, scope=ec)
            sc.dma("pool", woh[:], dd["w_oh"][l].rearrange("(k p) c -> p k c", p=128), writes=[woh.b])
            sc.dma("pool", wom[:], dd["w_om"][l].rearrange("(k p) c -> p k c", p=128), writes=[wom.b])
            wga = [TT(f"wga{i}", [128, KD, 128], BF16, scope=ec) for i in range(2)]
            wgb = [TT(f"wgb{i}", [128, KD, 128], BF16, scope=ec) for i in range(2)]
            sga = [TT(f"sga{i}", [128, 512], F32, scope=ec) for i in range(2)]
            sgb = [TT(f"sgb{i}", [128, 512], F32, scope=ec) for i in range(2)]
            ci_ = 0
            for m in range(KD):
                wa_ = wga[m % 2]
                wb_ = wgb[m % 2]
                wload(wa_, 3232 + m * 128, 128)
                wload(wb_, 3232 + 1024 + m * 128, 128)
                for blk in range(NB):
                    ts = slice(blk * 512, (blk + 1) * 512)
                    pya, pyb, pga, pgb = nextps(), nextps(), nextps(), nextps()

                    def mm4(e, pp, wt, src, nk, ts=ts, m=m, whole=False):
                        ins = None
                        for k in range(nk):
                            lhs = wt[:, k, :] if whole else wt[:, k, m * 128:(m + 1) * 128]
                            ins = e.matmul(pp[:, :], lhsT=lhs, rhs=src[:, k, ts], start=(k == 0), stop=(k == nk - 1))
                        return ins
                    sc.op("pe", lambda e, pp=pya: mm4(e, pp, woh, ogT, 4), reads=[woh.b, ogT.b], writes=[pya.b])
                    sc.op("pe", lambda e, pp=pyb: mm4(e, pp, wom, mlaT, 4), reads=[wom.b, mlaT.b], writes=[pyb.b])
                    sc.op("pe", lambda e, pp=pga, wa_=wa_: mm4(e, pp, wa_, hT, KD, whole=True), reads=[wa_.b, hT.b], writes=[pga.b])
                    sc.op("pe", lambda e, pp=pgb, wb_=wb_: mm4(e, pp, wb_, hT, KD, whole=True), reads=[wb_.b, hT.b], writes=[pgb.b])
                    a_ = sga[ci_ % 2]
                    b_ = sgb[ci_ % 2]
                    ci_ += 1
                    sc.op("act", lambda e, a_=a_, pga=pga: e.activation(out=a_[:], in_=pga[:, :], func=AF.Sigmoid), reads=[pga.b], writes=[a_.b])
                    sc.op("act", lambda e, b_=b_, pgb=pgb: e.activation(out=b_[:], in_=pgb[:, :], func=AF.Sigmoid), reads=[pgb.b], writes=[b_.b])
                    sc.op("dve", lambda e, a_=a_, pya=pya: e.tensor_tensor(out=a_[:], in0=pya[:, :], in1=a_[:], op=ALU.mult), reads=[pya.b, a_.b], writes=[a_.b])
                    sc.op("dve", lambda e, b_=b_, pyb=pyb: e.tensor_tensor(out=b_[:], in0=pyb[:, :], in1=b_[:], op=ALU.mult), reads=[pyb.b, b_.b], writes=[b_.b])
                    sc.op("pool", lambda e, a_=a_, b_=b_, m=m, ts=ts: e.tensor_tensor(out=yT[:, m, ts], in0=a_[:], in1=b_[:], op=ALU.add),
                          reads=[a_.b, b_.b], writes=[yT.b])
            wo = [TT(f"wo{i}", [128, KD, 128], BF16, scope=ec) for i in range(2)]
            for m in range(KD):
                w_ = wo[m % 2]
                sc.dma("pool", w_[:], dd["w_o"][l, :, m * 128:(m + 1) * 128].rearrange("(k p) c -> p k c", p=128), writes=[w_.b])
                for blk in range(NB):
                    ts = slice(blk * 512, (blk + 1) * 512)
                    ps = nextps()

                    def mmo2(e, ps=ps, w_=w_, ts=ts):
                        ins = None
                        for k in range(KD):
                            ins = e.matmul(ps[:, :], lhsT=w_[:, k, :], rhs=yT[:, k, ts], start=(k == 0), stop=(k == KD - 1))
                        return ins
                    sc.op("pe", mmo2, reads=[w_.b, yT.b], writes=[ps.b])
                    sc.op("dve", lambda e, ps=ps, m=m, ts=ts: e.scalar_tensor_tensor(
                        out=xT[:, m, ts], in0=ps[:, :], scalar=modT[:, l, 16 + m:16 + m + 1], in1=xT[:, m, ts],
                        op0=ALU.mult, op1=ALU.add), reads=[ps.b, modT.b, xb[m]], writes=[xb[m]])
            sc.barrier()
        sc.barrier()


def build_program(L, do_mixer=True, do_moe=True, do_final=True, dbg=None):
    nc = bass.Bass("TRN2", target_bir_lowering=False)

    def din(name, shape, dt=F32):
        return nc.dram_tensor(name, list(shape), dt, kind="ExternalInput").ap()

    x_d = din("x", [S, D])
    ccol_d = din("ccol", [128, KD])
    pos_d = din("posrep", [128, S], I32)
    w_ada_d = din("w_ada", [L, D, 6 * D])
    bada_d = din("bada_col", [128, L, 48])
    nmg_d = din("nmg_col", [128, L, KD])
    nfg_d = din("nfg_col", [128, L, KD])
    fng_d = din("fng_col", [128, KD])
    w_in_d = din("w_in", [L, D, INW])
    lbl_col_d = din("lbl_col", [128, 4, 8])
    lbl_row_d = din("lbl_row", [128, 4, 1024])
    hgain_d = din("hgain_row", [128, L, 128])
    w_oh_d = din("w_out_hgrn", [L, 512, D])
    qng_d = din("qng_col", [128, L, 3])
    w_qup_d = din("w_q_up", [L, 384, 768])
    kvng_d = din("kvng_col", [128, L, 2])
    w_kvup_d = din("w_kv_up", [L, 256, 1024])
    w_om_d = din("w_out_mla", [L, 512, D])
    w_o_d = din("w_o", [L, D, D])
    w_r_d = din("w_router", [L, D, E])
    br_d = din("br_row", [128, L, E])
    w_gu_d = din("w_gate_up", [L, E, D, 2 * D])
    bg_d = din("bg_col", [L, 128, E * 8])
    bu_d = din("bu_col", [L, 128, E * 8])
    w_dn_d = din("w_down", [L, E, D, D])
    bd_d = din("b_down", [L, E, D])
    ident_d = din("ident", [128, 128])
    uext_d = din("uext", [128, 2, 67])
    mask_d = din("maskd", [128, 2, 64])
    freq_d = din("freqcol", [128, 1])
    y_d = nc.dram_tensor("y", [S, D], F32, kind="ExternalOutput").ap()

    with ExitStack() as es:
        sc = Sched(nc, es)

        def TT(name, shape, dt, scope=es, psum=False):
            return T(sc, scope, name, shape, dt, psum=psum)

        xT = TT("xT", [128, KD, S], F32)
        xb = [sc.buf(f"xTb{k}") for k in range(KD)]
        hT = TT("hT", [128, KD, S], BF16)
        ident = TT("ident_sb", [128, 128], F32)
        ones_col = TT("ones_col", [128, 1], F32)
        ones_row = TT("ones_row", [1, 128], F32)
        modT = TT("modT", [128, L, 48], F32)
        gsc1 = TT("gsc1", [128, L, KD], F32)
        gsc2 = TT("gsc2", [128, L, KD], F32)
        ccol = TT("ccol_sb", [128, KD], F32)
        PS = [TT(f"ps{i}", [128, 512], F32, psum=True) for i in range(8)]
        psi = [0]

        def nextps():
            p = PS[psi[0] % 8]
            psi[0] += 1
            return p

        sc.dma("sp", ident[:], ident_d, writes=[ident.b])
        sc.op("dve", lambda e: e.memset(ones_col[:], 1.0), writes=[ones_col.b])
        sc.op("dve", lambda e: e.memset(ones_row[:], 1.0), writes=[ones_row.b])
        sc.dma("sp", ccol[:], ccol_d, writes=[ccol.b])
        sc.op("act", lambda e: e.activation(out=ccol[:], in_=ccol[:], func=AF.Silu), reads=[ccol.b], writes=[ccol.b])

        with ExitStack() as es0:
            wa = [TT(f"wada{i}", [128, KD, 768], F32, scope=es0) for i in range(2)]
            bada = TT("bada_sb", [128, L, 48], F32, scope=es0)
            nmg = TT("nmg_sb", [128, L, KD], F32, scope=es0)
            nfg = TT("nfg_sb", [128, L, KD], F32, scope=es0)
            sc.dma("sp", bada[:], bada_d, writes=[bada.b])
            sc.dma("sp", nmg[:], nmg_d, writes=[nmg.b])
            sc.dma("sp", nfg[:], nfg_d, writes=[nfg.b])
            gi = 0
            for l in range(L):
                for grp in range(8):
                    w = wa[gi % 2]
                    gi += 1
                    sc.dma("sp", w[:], w_ada_d[l, :, grp * 768:(grp + 1) * 768].rearrange("(k p) c -> p k c", p=128),
                           writes=[w.b])
                    ps = nextps()

                    def mm(e, w=w, ps=ps):
                        ins = None
                        for j in range(6):
                            for k in range(KD):
                                ins = e.matmul(ps[:, j:j + 1], lhsT=w[:, k, j * 128:(j + 1) * 128], rhs=ccol[:, k:k + 1],
                                               start=(k == 0), stop=(k == KD - 1))
                        return ins
                    sc.op("pe", mm, reads=[w.b, ccol.b], writes=[ps.b])
                    sc.op("dve", lambda e, ps=ps, l=l, grp=grp: e.tensor_tensor(
                        out=modT[:, l, grp * 6:(grp + 1) * 6], in0=ps[:, 0:6], in1=bada[:, l, grp * 6:(grp + 1) * 6], op=ALU.add),
                        reads=[ps.b, bada.b], writes=[modT.b])
            for l in range(L):
                sc.op("dve", lambda e, l=l: e.scalar_tensor_tensor(out=gsc1[:, l, :], in0=modT[:, l, 8:16], scalar=1.0, in1=nmg[:, l, :],
                                                                   op0=ALU.add, op1=ALU.mult), reads=[modT.b, nmg.b], writes=[gsc1.b])
                sc.op("dve", lambda e, l=l: e.scalar_tensor_tensor(out=gsc2[:, l, :], in0=modT[:, l, 32:40], scalar=1.0, in1=nfg[:, l, :],
                                                                   op0=ALU.add, op1=ALU.mult), reads=[modT.b, nfg.b], writes=[gsc2.b])
            sc.barrier()

        with ExitStack() as es0:
            xin = [TT(f"xin{i}", [128, D], F32, scope=es0) for i in range(2)]
            for t in range(NT):
                xi = xin[t % 2]
                sc.dma("sp", xi[:], x_d[t * 128:(t + 1) * 128, :], writes=[xi.b])
                for half in range(2):
                    ps = nextps()

                    def tr(e, xi=xi, ps=ps, half=half):
                        ins = None
                        for j in range(4):
                            k = half * 4 + j
                            ins = e.transpose(ps[:, j * 128:(j + 1) * 128], xi[:, k * 128:(k + 1) * 128], ident[:])
                        return ins
                    sc.op("pe", tr, reads=[xi.b, ident.b], writes=[ps.b])
                    wr_ = xb[half * 4:(half + 1) * 4]
                    if half == 0:
                        sc.op("dve", lambda e, ps=ps, half=half, t=t: e.tensor_copy(
                            out=xT[:, half * 4:(half + 1) * 4, t * 128:(t + 1) * 128],
                            in_=ps[:, :].rearrange("p (j c) -> p j c", c=128)), reads=[ps.b], writes=wr_)
                    else:
                        sc.op("act", lambda e, ps=ps, half=half, t=t: e.copy(
                            out=xT[:, half * 4:(half + 1) * 4, t * 128:(t + 1) * 128],
                            in_=ps[:, :].rearrange("p (j c) -> p j c", c=128)), reads=[ps.b], writes=wr_)
            sc.barrier()

        def rstd_block(ps, rrow, ts, sq, cntbox):
            for k in range(KD):
                q = sq[cntbox[0] % 2]
                cntbox[0] += 1
                sc.op("act", lambda e, q=q, k=k: e.activation(out=q[:], in_=xT[:, k, ts], func=AF.Square),
                      reads=[xb[k]], writes=[q.b])
                sc.op("pe", lambda e, q=q, k=k: e.matmul(ps[0:1, :], lhsT=ones_col[:], rhs=q[:],
                                                         start=(k == 0), stop=(k == KD - 1)),
                      reads=[q.b, ones_col.b], writes=[ps.b])
            sc.op("dve", lambda e: e.tensor_scalar(out=rrow[0:1, ts], in0=ps[0:1, :], scalar1=1.0 / D, scalar2=EPS,
                                                   op0=ALU.mult, op1=ALU.add), reads=[ps.b], writes=[rrow.b])
            sc.op("act", lambda e: e.activation(out=rrow[0:1, ts], in_=rrow[0:1, ts], func=AF.Sqrt),
                  reads=[rrow.b], writes=[rrow.b])
            sc.op("dve", lambda e: e.reciprocal(out=rrow[0:1, ts], in_=rrow[0:1, ts]), reads=[rrow.b], writes=[rrow.b])

        def norm_modulate(l, gsc, sh_off, h32_cb=None):
            with ExitStack() as es1:
                sq = [TT(f"sq{i}", [128, 512], F32, scope=es1) for i in range(2)]
                rrow = TT("rrow", [1, S], F32, scope=es1)
                tmp = [TT(f"ntmp{i}", [128, 512], F32, scope=es1) for i in range(2)]
                h32 = TT("h32", [128, KD, 512], F32, scope=es1) if h32_cb is not None else None
                cntbox = [0]
                for blk in range(NB):
                    ts = slice(blk * 512, (blk + 1) * 512)
                    ps = nextps()
                    rstd_block(ps, rrow, ts, sq, cntbox)
                    pb = nextps()
                    sc.op("pe", lambda e, pb=pb, ts=ts: e.matmul(pb[:, :], lhsT=ones_row[:], rhs=rrow[0:1, ts], start=True, stop=True),
                          reads=[rrow.b, ones_row.b], writes=[pb.b])
                    for k in range(KD):
                        tm = tmp[cntbox[0] % 2]
                        cntbox[0] += 1
                        sc.op("dve", lambda e, tm=tm, k=k, ts=ts, pb=pb: e.tensor_tensor(out=tm[:], in0=xT[:, k, ts], in1=pb[:, :], op=ALU.mult),
                              reads=[xb[k], pb.b], writes=[tm.b])
                        if h32 is None:
                            sc.op("act", lambda e, tm=tm, k=k, ts=ts: e.activation(
                                out=hT[:, k, ts], in_=tm[:], func=AF.Identity, scale=gsc[:, l, k:k + 1],
                                bias=modT[:, l, sh_off + k:sh_off + k + 1]),
                                reads=[tm.b, gsc.b, modT.b], writes=[hT.b])
                        else:
                            sc.op("act", lambda e, tm=tm, k=k: e.activation(
                                out=h32[:, k, :], in_=tm[:], func=AF.Identity, scale=gsc[:, l, k:k + 1],
                                bias=modT[:, l, sh_off + k:sh_off + k + 1]),
                                reads=[tm.b, gsc.b, modT.b], writes=[h32.b])
                    if h32 is not None:
                        sc.op("pool", lambda e, ts=ts: e.tensor_copy(out=hT[:, :, ts], in_=h32[:, :, :]), reads=[h32.b], writes=[hT.b])
                        h32_cb(blk, h32)
                sc.barrier()

        def moe_layer(l):
            g2off = 40
            with ExitStack() as es1:
                wr = TT("wr_sb", [128, KD, E], F32, scope=es1)
                brr = TT("br_sb", [128, E], F32, scope=es1)
                wT = TT("wT_sb", [E, S], F32, scope=es1)
                bdn = TT("bdn_sb", [E, D], F32, scope=es1)
                bg = TT("bg_sb", [128, E * 8], F32, scope=es1)
                bu = TT("bu_sb", [128, E * 8], F32, scope=es1)
                sc.dma("sp", wr[:], w_r_d[l].rearrange("(k p) e -> p k e", p=128), writes=[wr.b])
                sc.dma("sp", brr[:], br_d[:, l, :], writes=[brr.b])
                sc.dma("sp", bdn[:], bd_d[l], writes=[bdn.b])
                sc.dma("sp", bg[:], bg_d[l], writes=[bg.b])
                sc.dma("sp", bu[:], bu_d[l], writes=[bu.b])
                sc.op("dve", lambda e: e.tensor_scalar(out=bu[:], in0=bu[:], scalar1=1.0, scalar2=None, op0=ALU.add),
                      reads=[bu.b], writes=[bu.b])
                lg = TT("lg", [128, E], F32, scope=es1)
                top8 = TT("top8", [128, 8], F32, scope=es1)
                negm = TT("negm", [128, 1], F32, scope=es1)
                ex = TT("ex", [128, E], F32, scope=es1)
                ssum = TT("ssum", [128, 1], F32, scope=es1)
                wtm = TT("wtm", [128, E], F32, scope=es1)

                def router(blk, h32):
                    for j in range(4):
                        t = blk * 4 + j
                        ps = nextps()

                        def mm(e, ps=ps, j=j):
                            ins = None
                            for k in range(KD):
                                ins = e.matmul(ps[:, 0:E], lhsT=h32[:, k, j * 128:(j + 1) * 128], rhs=wr[:, k, :],
                                               start=(k == 0), stop=(k == KD - 1))
                            return ins
                        sc.op("pe", mm, reads=[h32.b, wr.b], writes=[ps.b])
                        sc.op("dve", lambda e, ps=ps: e.tensor_tensor(out=lg[:], in0=ps[:, 0:E], in1=brr[:], op=ALU.add),
                              reads=[ps.b, brr.b], writes=[lg.b])
                        sc.op("dve", lambda e: e.max(out=top8[:], in_=lg[:]), reads=[lg.b], writes=[top8.b])
                        sc.op("dve", lambda e: e.tensor_scalar(out=negm[:], in0=top8[:, 0:1], scalar1=-1.0, scalar2=None, op0=ALU.mult),
                              reads=[top8.b], writes=[negm.b])
                        sc.op("act", lambda e: e.activation(out=ex[:], in_=lg[:], func=AF.Exp, bias=negm[:, 0:1], scale=1.0),
                              reads=[lg.b, negm.b], writes=[ex.b])
                        sc.op("dve", lambda e: e.tensor_scalar(out=lg[:], in0=lg[:], scalar1=top8[:, 3:4], scalar2=None, op0=ALU.is_ge),
                              reads=[lg.b, top8.b, ex.b], writes=[lg.b])
                        sc.op("dve", lambda e: e.tensor_tensor(out=wtm[:], in0=ex[:], in1=lg[:], op=ALU.mult),
                              reads=[ex.b, lg.b], writes=[wtm.b])
                        sc.op("dve", lambda e: e.reduce_sum(out=ssum[:], in_=wtm[:], axis=AX.X), reads=[wtm.b], writes=[ssum.b])
                        sc.op("dve", lambda e: e.reciprocal(out=ssum[:], in_=ssum[:]), reads=[ssum.b], writes=[ssum.b])
                        sc.op("dve", lambda e: e.tensor_scalar(out=wtm[:], in0=wtm[:], scalar1=ssum[:, 0:1], scalar2=None, op0=ALU.mult),
                              reads=[wtm.b, ssum.b], writes=[wtm.b])
                        pt = nextps()
                        sc.op("pe", lambda e, pt=pt: e.transpose(pt[0:E, 0:128], wtm[:], ident[:]), reads=[wtm.b, ident.b], writes=[pt.b])
                        sc.op("act", lambda e, pt=pt, t=t: e.copy(out=wT[:, t * 128:(t + 1) * 128], in_=pt[0:E, 0:128]),
                              reads=[pt.b], writes=[wT.b])

                norm_modulate(l, gsc2, 24, h32_cb=router)

                for blk in range(NB):
                    ts = slice(blk * 512, (blk + 1) * 512)
                    for m in range(KD):
                        ps = nextps()
                        sc.op("pe", lambda e, ps=ps, m=m, ts=ts: e.matmul(ps[:, :], lhsT=bdn[:, m * 128:(m + 1) * 128], rhs=wT[:, ts],
                                                                          start=True, stop=True), reads=[bdn.b, wT.b], writes=[ps.b])
                        sc.op("dve", lambda e, ps=ps, m=m, ts=ts: e.scalar_tensor_tensor(
                            out=xT[:, m, ts], in0=ps[:, :], scalar=modT[:, l, g2off + m:g2off + m + 1], in1=xT[:, m, ts],
                            op0=ALU.mult, op1=ALU.add), reads=[ps.b, modT.b, xb[m]], writes=[xb[m]])

                NU = 2
                wgu = [TT(f"wgu{i}", [128, KD, 1024], BF16, scope=es1) for i in range(NU)]
                wdn = [TT(f"wdn{i}", [128, 4, D], BF16, scope=es1) for i in range(NU)]
                act = TT("actb", [128, 4, S], BF16, scope=es1)
                actb = [sc.buf(f"actblk{i}") for i in range(NB)]
                wbc = [TT(f"wbc{i}", [128, S], BF16, scope=es1) for i in range(2)]
                gt = [TT(f"gt{i}", [128, 512], F32, scope=es1) for i in range(2)]
                ut = [TT(f"ut{i}", [128, 512], F32, scope=es1) for i in range(2)]
                st = [TT(f"st{i}", [128, 512], F32, scope=es1) for i in range(2)]
                pc = 0
                NUNITS = E * 2

                def load_unit(u):
                    e_, hf = divmod(u, 2)
                    wg = wgu[u % NU]
                    wd = wdn[u % NU]
                    sc.dma("pool", wg[:], w_gu_d[l, e_, :, hf * 1024:(hf + 1) * 1024].rearrange("(k p) c -> p k c", p=128),
                           writes=[wg.b])
                    sc.dma("pool", wd[:], w_dn_d[l, e_, hf * 512:(hf + 1) * 512, :].rearrange("(c p) n -> p c n", p=128),
                           writes=[wd.b])

                load_unit(0)
                load_unit(1)
                for e_ in range(E):
                    wb = wbc[e_ % 2]
                    for blk in range(NB):
                        ts = slice(blk * 512, (blk + 1) * 512)
                        ps = nextps()
                        sc.op("pe", lambda e, ps=ps, ts=ts, e_=e_: e.matmul(
                            ps[:, :], lhsT=ident[0:E, e_:e_ + 1].to_broadcast([E, 128]), rhs=wT[:, ts],
                            start=True, stop=True), reads=[ident.b, wT.b], writes=[ps.b])
                        sc.op("act", lambda e, ps=ps, ts=ts, wb=wb: e.copy(out=wb[:, ts], in_=ps[:, :]), reads=[ps.b], writes=[wb.b])
                    for hf in range(2):
                        u = e_ * 2 + hf
                        wg = wgu[u % NU]
                        wd = wdn[u % NU]
                        pending = []
                        for blk in range(NB):
                            ts = slice(blk * 512, (blk + 1) * 512)
                            for c in range(4):
                                fc = hf * 4 + c
                                bcol = e_ * 8 + fc
                                pg = nextps()
                                pu = nextps()

                                def mmg(e, pp, off, wg=wg, c=c, ts=ts):
                                    ins = None
                                    for k in range(KD):
                                        ins = e.matmul(pp[:, :], lhsT=wg[:, k, c * 256 + off:c * 256 + 256:2], rhs=hT[:, k, ts],
                                                       start=(k == 0), stop=(k == KD - 1))
                                    return ins
                                sc.op("pe", lambda e, pg=pg: mmg(e, pg, 0), reads=[wg.b, hT.b], writes=[pg.b])
                                sc.op("pe", lambda e, pu=pu: mmg(e, pu, 1), reads=[wg.b, hT.b], writes=[pu.b])
                                g = gt[pc % 2]
                                uu = ut[pc % 2]
                                s_ = st[pc % 2]
                                pc += 1
                                sc.op("dve", lambda e, g=g, pg=pg, bcol=bcol: e.tensor_scalar(
                                    out=g[:], in0=pg[:, :], scalar1=bg[:, bcol:bcol + 1], scalar2=7.0, op0=ALU.add, op1=ALU.min),
                                    reads=[pg.b, bg.b], writes=[g.b])
                                for fn in pending:
                                    fn()
                                pending = []
                                sc.op("dve", lambda e, uu=uu, pu=pu, bcol=bcol: e.tensor_scalar(
                                    out=uu[:], in0=pu[:, :], scalar1=bu[:, bcol:bcol + 1], scalar2=-6.0, op0=ALU.add, op1=ALU.max),
                                    reads=[pu.b, bu.b], writes=[uu.b])
                                sc.op("act", lambda e, s_=s_, g=g: e.activation(out=s_[:], in_=g[:], func=AF.Sigmoid, scale=1.702),
                                      reads=[g.b], writes=[s_.b])
                                sc.op("pool", lambda e, s_=s_, g=g: e.tensor_tensor(out=s_[:], in0=s_[:], in1=g[:], op=ALU.mult),
                                      reads=[s_.b, g.b], writes=[s_.b])
                                sc.op("pool", lambda e, s_=s_, wb=wb, ts=ts: e.tensor_tensor(out=s_[:], in0=s_[:], in1=wb[:, ts], op=ALU.mult),
                                      reads=[s_.b, wb.b], writes=[s_.b])

                                def fin(s_=s_, uu=uu, c=c, ts=ts, blk=blk):
                                    sc.op("dve", lambda e: e.scalar_tensor_tensor(
                                        out=act[:, c, ts], in0=uu[:], scalar=8.0, in1=s_[:], op0=ALU.min, op1=ALU.mult),
                                        reads=[s_.b, uu.b], writes=[actb[blk]])
                                pending.append(fin)
                        for fn in pending:
                            fn()
                        pending = []
                        for blk in range(NB):
                            ts = slice(blk * 512, (blk + 1) * 512)
                            for m in range(KD):
                                po = nextps()

                                def mmd(e, po=po, wd=wd, m=m, ts=ts):
                                    ins = None
                                    for c in range(4):
                                        ins = e.matmul(po[:, :], lhsT=wd[:, c, m * 128:(m + 1) * 128], rhs=act[:, c, ts],
                                                       start=(c == 0), stop=(c == 3))
                                    return ins
                                sc.op("pe", mmd, reads=[wd.b, actb[blk]], writes=[po.b])
                                sc.op("dve", lambda e, po=po, m=m, ts=ts: e.scalar_tensor_tensor(
                                    out=xT[:, m, ts], in0=po[:, :], scalar=modT[:, l, g2off + m:g2off + m + 1], in1=xT[:, m, ts],
                                    op0=ALU.mult, op1=ALU.add), reads=[po.b, modT.b, xb[m]], writes=[xb[m]])
                        if u + 2 < NUNITS:
                            load_unit(u + 2)
                sc.barrier()

        for l in range(L):
            if do_mixer:
                mixer_layer(nc, sc, TT, nextps, PS, l, xT, xb, hT, ident, modT, gsc1, norm_modulate,
                            dict(w_in=w_in_d, lbl_col=lbl_col_d, lbl_row=lbl_row_d, hgain=hgain_d, w_oh=w_oh_d, qng=qng_d,
                                 w_qup=w_qup_d, kvng=kvng_d, w_kvup=w_kvup_d, w_om=w_om_d, w_o=w_o_d, uext=uext_d, mask=mask_d,
                                 freq=freq_d, pos=pos_d), dbg)
            if do_moe:
                moe_layer(l)

        with ExitStack() as es1:
            fng = TT("fng_sb", [128, KD], F32, scope=es1)
            sc.dma("sp", fng[:], fng_d, writes=[fng.b])
            rrow = TT("rrowf", [1, S], F32, scope=es1)
            sq = [TT(f"sqf{i}", [128, 512], F32, scope=es1) for i in range(2)]
            xn = TT("xn", [128, KD, 512], F32, scope=es1)
            yo = [TT(f"yo{i}", [128, D], F32, scope=es1) for i in range(2)]
            ybufs = [sc.buf("yd0"), sc.buf("yd1")]
            cntbox = [0]
            for blk in range(NB):
                ts = slice(blk * 512, (blk + 1) * 512)
                if do_final:
                    ps = nextps()
                    rstd_block(ps, rrow, ts, sq, cntbox)
                    pb = nextps()
                    sc.op("pe", lambda e, pb=pb, ts=ts: e.matmul(pb[:, :], lhsT=ones_row[:], rhs=rrow[0:1, ts], start=True, stop=True),
                          reads=[rrow.b, ones_row.b], writes=[pb.b])
                    for k in range(KD):
                        sc.op("dve", lambda e, k=k, ts=ts, pb=pb: e.scalar_tensor_tensor(
                            out=xn[:, k, :], in0=xT[:, k, ts], scalar=fng[:, k:k + 1], in1=pb[:, :], op0=ALU.mult, op1=ALU.mult),
                            reads=[xb[k], pb.b, fng.b], writes=[xn.b])
                else:
                    sc.op("dve", lambda e, ts=ts: e.tensor_copy(out=xn[:, :, :], in_=xT[:, :, ts]), reads=xb, writes=[xn.b])
                for j in range(4):
                    t = blk * 4 + j
                    y = yo[t % 2]
                    for half in range(2):
                        ps = nextps()

                        def tr(e, ps=ps, half=half, j=j):
                            ins = None
                            for jj in range(4):
                                k = half * 4 + jj
                                ins = e.transpose(ps[:, jj * 128:(jj + 1) * 128], xn[:, k, j * 128:(j + 1) * 128], ident[:])
                            return ins
                        sc.op("pe", tr, reads=[xn.b, ident.b], writes=[ps.b])
                        if half == 0:
                            sc.op("dve", lambda e, ps=ps, y=y, half=half: e.tensor_copy(out=y[:, half * 512:(half + 1) * 512], in_=ps[:, :]),
                                  reads=[ps.b], writes=[y.b])
                        else:
                            sc.op("act", lambda e, ps=ps, y=y, half=half: e.copy(out=y[:, half * 512:(half + 1) * 512], in_=ps[:, :]),
                                  reads=[ps.b], writes=[y.b])
                    sc.dma("sp", y_d[t * 128:(t + 1) * 128, :], y[:], reads=[y.b], writes=[ybufs[t % 2]])
            sc.barrier()
    return nc


def _consts():
    s = np.arange(64)
    uext = np.zeros((2, 64, 67), np.float32)
    uext[0, :, :64] = (s[:, None] <= s[None, :]).astype(np.float32) - (s[:, None] <= 31).astype(np.float32)
    uext[0, :, 64] = (s <= 31)
    uext[0, :, 65] = 1.0
    uext[0, :, 66] = (s > 31)
    uext[1, :, :64] = (s[:, None] >= s[None, :]).astype(np.float32) - (s[:, None] >= 32).astype(np.float32)
    uext[1, :, 64] = (s >= 32)
    uext[1, :, 65] = 1.0
    uext[1, :, 66] = (s < 32)
    uext = np.concatenate([uext, uext], axis=1)
    mask = np.zeros((2, 64, 64), np.float32)
    mask[0] = (s[:, None] <= s[None, :])
    mask[1] = (s[:, None] >= s[None, :])
    mask = np.concatenate([mask, mask], axis=1)
    freq = np.zeros((128, 1), np.float32)
    inv = (10000.0 ** (-np.arange(0, 32, 2, dtype=np.float32) / 32.0)).astype(np.float32)
    freq[64:80, 0] = inv / np.float32(2 * np.pi)
    freq[80:96, 0] = inv / np.float32(2 * np.pi)
    return dict(ident=np.eye(128, dtype=np.float32),
                uext=np.ascontiguousarray(uext.transpose(1, 0, 2)),
                maskd=np.ascontiguousarray(mask.transpose(1, 0, 2)),
                freqcol=freq)


def _prep(inputs, L, b):
    f = lambda a: np.ascontiguousarray(np.asarray(a, dtype=np.float32))
    col = lambda v, n: np.ascontiguousarray(v.reshape(v.shape[0], n, 128).transpose(2, 0, 1))
    m = {}
    m["x"] = f(inputs["x"][b])
    m["ccol"] = np.ascontiguousarray(f(inputs["c"][b]).reshape(KD, 128).T)
    m["posrep"] = np.ascontiguousarray(np.broadcast_to(np.asarray(inputs["positions"][b], dtype=np.int32)[None, :], (128, S)))
    m["w_ada"] = f(inputs["w_ada"][:L])
    m["bada_col"] = col(f(inputs["b_ada"][:L]), 48)
    m["nmg_col"] = col(f(inputs["norm_mix_gain"][:L]), KD)
    m["nfg_col"] = col(f(inputs["norm_ffn_gain"][:L]), KD)
    m["fng_col"] = np.ascontiguousarray(f(inputs["final_norm_gain"]).reshape(KD, 128).T)
    m["w_in"] = f(inputs["w_in"][:L])
    lbl = f(inputs["hgrn_lb_logits"])
    m["lbl_col"] = np.ascontiguousarray(lbl.reshape(4, 8, 128).transpose(2, 0, 1))
    m["lbl_row"] = np.ascontiguousarray(np.broadcast_to(lbl.reshape(1, 4, 1024), (128, 4, 1024)))
    m["hgain_row"] = np.ascontiguousarray(np.broadcast_to(f(inputs["hgrn_out_norm_gain"][:L])[None], (128, L, 128)))
    m["w_out_hgrn"] = f(inputs["w_out_hgrn"][:L])
    m["qng_col"] = col(f(inputs["mla_q_norm_gain"][:L]), 3)
    m["w_q_up"] = f(inputs["w_q_up"][:L])
    m["kvng_col"] = col(f(inputs["mla_kv_norm_gain"][:L]), 2)
    m["w_kv_up"] = f(inputs["w_kv_up"][:L])
    m["w_out_mla"] = f(inputs["w_out_mla"][:L])
    m["w_o"] = f(inputs["w_o"][:L])
    m["w_router"] = f(inputs["w_router"][:L])
    m["br_row"] = np.ascontiguousarray(np.broadcast_to(f(inputs["b_router"][:L])[None], (128, L, E)))
    m["w_gate_up"] = f(inputs["w_gate_up"][:L])
    bgu = f(inputs["b_gate_up"][:L])
    bgc = bgu[:, :, 0::2].reshape(L, E, 8, 128).transpose(0, 3, 1, 2).reshape(L, 128, E * 8)
    buc = bgu[:, :, 1::2].reshape(L, E, 8, 128).transpose(0, 3, 1, 2).reshape(L, 128, E * 8)
    m["bg_col"] = np.ascontiguousarray(bgc)
    m["bu_col"] = np.ascontiguousarray(buc)
    m["w_down"] = f(inputs["w_down"][:L])
    m["b_down"] = f(inputs["b_down"][:L])
    return m


_PROG = {}


def kernel(**inputs):
    L = 4
    nb = 8
    if "full" not in _PROG:
        _PROG["full"] = build_program(L)
    nc = _PROG["full"]
    consts = _consts()
    shared = None
    in_maps = []
    for b in range(nb):
        m = _prep(inputs, L, b)
        if shared is None:
            shared = {k: v for k, v in m.items() if k not in ("x", "ccol", "posrep")}
        else:
            for k in shared:
                m[k] = shared[k]
        m.update(consts)
        in_maps.append(m)
    res = run_bass_kernel_spmd(nc, in_maps, core_ids=list(range(nb)))
    out = np.stack([np.asarray(r["y"], dtype=np.float32) for r in res.results], axis=0)
    return out
```
